# Optimizing a Trainium2 kernel written in Bass

```python
import jax, jax.numpy as jnp
from jax import lax
import numpy as np

D_MODEL = 4096
BATCH = 2
SEQ = 4096
DEPTH = 2

MIX_WIDTH = D_MODEL
CONV_WIDTH = D_MODEL // 2
ATTN_WIDTH = MIX_WIDTH - CONV_WIDTH
HEAD_DIM = 128
N_HEADS = ATTN_WIDTH // HEAD_DIM
N_KV_HEADS = 4
KV_WIDTH = N_KV_HEADS * HEAD_DIM
GROUP = N_HEADS // N_KV_HEADS
IDX_HEADS = 32
IDX_DIM = 64
TOPK_MAX = 256
CONV_KERNEL = 31
D_FF = 2 * D_MODEL
FFN_RES = 0.5
Q_BLOCK = 128
N_MOD = 9
EPS = 1e-6
COL_SIZES = (CONV_WIDTH, CONV_WIDTH, ATTN_WIDTH, KV_WIDTH, KV_WIDTH,
             IDX_HEADS * IDX_DIM, IDX_DIM, IDX_HEADS)
N_IN = int(sum(COL_SIZES))

kernel_name = "hymba_conformer_dsa_macaron_adaln"


def _rmsnorm(x, g):
    xf = x.astype(jnp.float32)
    y = xf * lax.rsqrt(jnp.mean(xf * xf, axis=-1, keepdims=True) + EPS)
    return (y * g.astype(jnp.float32)).astype(x.dtype)


def _layernorm(x, g, b):
    xf = x.astype(jnp.float32)
    mu = jnp.mean(xf, axis=-1, keepdims=True)
    var = jnp.mean(jnp.square(xf - mu), axis=-1, keepdims=True)
    y = (xf - mu) * lax.rsqrt(var + EPS)
    return (y * g.astype(jnp.float32) + b.astype(jnp.float32)).astype(x.dtype)


def _modulate(h, shift, scale):
    return h * (1.0 + scale[:, None, :]) + shift[:, None, :]


def _swiglu(h, w_gu, w_d):
    g, u = jnp.split(h @ w_gu, 2, axis=-1)
    return (jax.nn.silu(g) * u) @ w_d


def _conformer_conv(a, gate, conv_w, conv_b, ln_g, ln_b):
    u = a * jax.nn.sigmoid(gate)
    y = lax.conv_general_dilated(
        u, conv_w[:, None, :].astype(u.dtype), window_strides=(1,),
        padding=[(CONV_KERNEL - 1, 0)],
        dimension_numbers=("NWC", "WIO", "NWC"),
        feature_group_count=CONV_WIDTH) + conv_b
    return jax.nn.silu(_layernorm(y, ln_g, ln_b))


def _dsa_attention(q, k, v, q_idx, k_idx, w_idx):
    B, T = q.shape[0], q.shape[1]
    topk = min(TOPK_MAX, T // 4)
    nblk = T // Q_BLOCK
    idx_scale = (IDX_DIM ** -0.5) * (IDX_HEADS ** -0.5)
    attn_scale = HEAD_DIM ** -0.5
    key_pos = jnp.arange(T, dtype=jnp.int32)
    k_idx_f = k_idx.astype(jnp.float32)

    def to_blocks(t):
        return jnp.swapaxes(t.reshape((B, nblk, Q_BLOCK) + t.shape[2:]), 0, 1)

    xs = (to_blocks(q), to_blocks(q_idx), to_blocks(w_idx),
          key_pos.reshape(nblk, Q_BLOCK))

    def block(args):
        qb, qib, wb, posb = args
        s_h = jnp.einsum("bqhd,bsd->bqhs", qib.astype(jnp.float32), k_idx_f)
        score = jnp.einsum("bqhs,bqh->bqs", jax.nn.relu(s_h),
                           wb.astype(jnp.float32)) * idx_scale
        causal = key_pos[None, None, :] <= posb[None, :, None]
        score = jnp.where(causal, score, -jnp.inf)
        _, sel = lax.top_k(score, topk)
        valid = sel <= posb[None, :, None]
        k_sel = jax.vmap(lambda kk, ii: kk[ii])(k, sel)
        v_sel = jax.vmap(lambda vv, ii: vv[ii])(v, sel)
        qg = qb.reshape(B, Q_BLOCK, N_KV_HEADS, GROUP, HEAD_DIM)
        logits = jnp.einsum("bqhgd,bqkhd->bqhgk", qg.astype(jnp.float32),
                            k_sel.astype(jnp.float32)) * attn_scale
        logits = jnp.where(valid[:, :, None, None, :], logits, -jnp.inf)
        p = jax.nn.softmax(logits, axis=-1)
        o = jnp.einsum("bqhgk,bqkhd->bqhgd", p.astype(v.dtype), v_sel)
        return o.reshape(B, Q_BLOCK, ATTN_WIDTH)

    out = lax.map(block, xs)
    return jnp.swapaxes(out, 0, 1).reshape(B, T, ATTN_WIDTH)


def _token_mix(h, w_in, conv_w, conv_b, conv_ln_g, conv_ln_b,
               conv_out_norm, attn_out_norm, w_out):
    B, T, _ = h.shape
    offsets = np.cumsum(COL_SIZES)[:-1].tolist()
    (a, gate, q, k, v, qi, ki, wi) = jnp.split(h @ w_in, offsets, axis=-1)
    y_conv = _conformer_conv(a, gate, conv_w, conv_b, conv_ln_g, conv_ln_b)
    y_attn = _dsa_attention(
        q.reshape(B, T, N_HEADS, HEAD_DIM),
        k.reshape(B, T, N_KV_HEADS, HEAD_DIM),
        v.reshape(B, T, N_KV_HEADS, HEAD_DIM),
        qi.reshape(B, T, IDX_HEADS, IDX_DIM), ki, wi)
    y = jnp.concatenate([_rmsnorm(y_conv, conv_out_norm),
                         _rmsnorm(y_attn, attn_out_norm)], axis=-1)
    return y @ w_out


def setup_inputs(seed: int = 0) -> dict:
    key = jax.random.key(seed)
    ks = jax.random.split(key, 24)
    f32 = jnp.float32
    L = DEPTH

    def nrm(k, shape, scale):
        return jax.random.normal(k, shape, f32) * scale

    def gain(k, shape):
        return 1.0 + 0.01 * jax.random.normal(k, shape, f32)

    return {
        "x": nrm(ks[0], (BATCH, SEQ, D_MODEL), 1.0),
        "c": nrm(ks[1], (BATCH, D_MODEL), 1.0),
        "ada_w": nrm(ks[2], (L, D_MODEL, N_MOD * D_MODEL), 0.5 * D_MODEL ** -0.5),
        "ada_b": nrm(ks[3], (L, N_MOD * D_MODEL), 0.01),
        "ffn1_norm": gain(ks[4], (L, D_MODEL)),
        "ffn1_wgu": nrm(ks[5], (L, D_MODEL, 2 * D_FF), D_MODEL ** -0.5),
        "ffn1_wd": nrm(ks[6], (L, D_FF, D_MODEL), D_FF ** -0.5),
        "mix_norm": gain(ks[7], (L, D_MODEL)),
        "w_in": nrm(ks[8], (L, D_MODEL, N_IN), D_MODEL ** -0.5),
        "conv_w": nrm(ks[9], (L, CONV_KERNEL, CONV_WIDTH), CONV_KERNEL ** -0.5),
        "conv_b": nrm(ks[10], (L, CONV_WIDTH), 0.01),
        "conv_ln_g": gain(ks[11], (L, CONV_WIDTH)),
        "conv_ln_b": nrm(ks[12], (L, CONV_WIDTH), 0.01),
        "conv_out_norm": gain(ks[13], (L, CONV_WIDTH)),
        "attn_out_norm": gain(ks[14], (L, ATTN_WIDTH)),
        "w_out": nrm(ks[15], (L, MIX_WIDTH, D_MODEL), MIX_WIDTH ** -0.5),
        "ffn2_norm": gain(ks[16], (L, D_MODEL)),
        "ffn2_wgu": nrm(ks[17], (L, D_MODEL, 2 * D_FF), D_MODEL ** -0.5),
        "ffn2_wd": nrm(ks[18], (L, D_FF, D_MODEL), D_FF ** -0.5),
        "final_norm": gain(ks[19], (D_MODEL,)),
    }


def reference(x, c, ada_w, ada_b, ffn1_norm, ffn1_wgu, ffn1_wd, mix_norm, w_in,
              conv_w, conv_b, conv_ln_g, conv_ln_b, conv_out_norm, attn_out_norm,
              w_out, ffn2_norm, ffn2_wgu, ffn2_wd, final_norm):
    c_act = jax.nn.silu(c)
    for l in range(DEPTH):
        mod = c_act @ ada_w[l] + ada_b[l]
        (sh1, sc1, g1, sh2, sc2, g2, sh3, sc3, g3) = jnp.split(mod, N_MOD, axis=-1)
        h = _modulate(_rmsnorm(x, ffn1_norm[l]), sh1, sc1)
        x = x + FFN_RES * g1[:, None, :] * _swiglu(h, ffn1_wgu[l], ffn1_wd[l])
        h = _modulate(_rmsnorm(x, mix_norm[l]), sh2, sc2)
        x = x + g2[:, None, :] * _token_mix(h, w_in[l], conv_w[l], conv_b[l],
                                            conv_ln_g[l], conv_ln_b[l],
                                            conv_out_norm[l], attn_out_norm[l],
                                            w_out[l])
        h = _modulate(_rmsnorm(x, ffn2_norm[l]), sh3, sc3)
        x = x + FFN_RES * g3[:, None, :] * _swiglu(h, ffn2_wgu[l], ffn2_wd[l])
    return _rmsnorm(x, final_norm)
```

```python
import numpy as np
from contextlib import ExitStack
import concourse.bass as bass
import concourse.mybir as mybir
from concourse.bass_utils import run_bass_kernel_spmd

F32 = mybir.dt.float32
BF16 = mybir.dt.bfloat16
AF = mybir.ActivationFunctionType
ALU = mybir.AluOpType
EPS = 1e-6
NEG = -1.0e30

FULL = dict(T=1024, D=4096, DFF=8192, CW=2048, AW=2048, NH=16, NKV=4, IH=32, IDIM=64, TOPK=256, CK=31,
            DEPTH=2, GT=1024, NCG=4, NB=2)


class Sem:
    def __init__(self, h):
        self.h = h
        self.n = 0


class Prog:
    def __init__(self, nc, stack):
        self.nc = nc
        self.stack = stack
        self.q = {k: [] for k in ("sp", "act", "pe", "dve", "pool")}
        self.waited = {}

    def sem(self, name):
        return Sem(self.stack.enter_context(self.nc.semaphore(name)))

    def sb(self, name, shape, dt):
        return self.stack.enter_context(self.nc.sbuf_tensor(name, shape, dt))

    def ps(self, name, shape, dt):
        return self.stack.enter_context(self.nc.psum_tensor(name, shape, dt))

    def op(self, eng, fn, sem=None, dma=False):
        if sem is not None:
            inc = 16 if dma else 1
            sem.n += inc
            self.q[eng].append(lambda e, fn=fn, h=sem.h, inc=inc: fn(e).then_inc(h, inc))
            return sem.n
        self.q[eng].append(lambda e, fn=fn: fn(e))
        return None

    def wait(self, eng, sem, val):
        if val is None or val <= 0:
            return
        key = (eng, id(sem))
        if self.waited.get(key, 0) >= val:
            return
        self.waited[key] = val
        self.q[eng].append(lambda e, h=sem.h, v=val: e.wait_ge(h, v))

    def emit(self):
        q = self.q
        with self.nc.Block() as block:
            @block.sync
            def _(e):
                for f in q["sp"]:
                    f(e)

            @block.scalar
            def _(e):
                for f in q["act"]:
                    f(e)

            @block.tensor
            def _(e):
                for f in q["pe"]:
                    f(e)

            @block.vector
            def _(e):
                for f in q["dve"]:
                    f(e)

            @block.gpsimd
            def _(e):
                for f in q["pool"]:
                    f(e)


class MK:
    def __init__(self, cfg):
        self.c = cfg
        c = cfg
        T, D, DFF = c["T"], c["D"], c["DFF"]
        self.KVW = c["NKV"] * 128
        self.QIW = c["IH"] * c["IDIM"]
        self.NIN = 2 * c["CW"] + c["AW"] + 2 * self.KVW + self.QIW + c["IDIM"] + c["IH"]
        L = c["DEPTH"]
        nc = self.nc = bass.Bass("TRN2", target_bir_lowering=False)
        dt = nc.dram_tensor
        self.x = dt("x", [T, D], F32, kind="ExternalInput").ap()
        self.cin = dt("c", [1, D], F32, kind="ExternalInput").ap()
        NCG = c["NCG"]
        TK = self.TK = T * NCG
        PAD = c["CK"] - 1
        self.ada_w = dt("ada_w", [L, D, 9 * D // NCG], F32, kind="ExternalInput").ap()
        self.ada_b = dt("ada_b", [L, 9 * D // NCG], F32, kind="ExternalInput").ap()
        self.onehotd = dt("onehot", [128, NCG], F32, kind="ExternalInput").ap()
        self.norms = dt("norms", [L, 3, D], F32, kind="ExternalInput").ap()
        self.wgu = [dt(f"ffn{i}_wgu", [L, D, 2 * DFF], F32, kind="ExternalInput").ap() for i in (1, 2)]
        self.wd = [dt(f"ffn{i}_wd", [L, DFF, D], F32, kind="ExternalInput").ap() for i in (1, 2)]
        self.w_in = dt("w_in", [L, D, self.NIN], F32, kind="ExternalInput").ap()
        self.conv_w = dt("conv_w", [L, c["CK"], c["CW"]], F32, kind="ExternalInput").ap()
        self.cvec = dt("cvec", [L, 4, c["CW"]], F32, kind="ExternalInput").ap()
        self.anorm = dt("attn_out_norm", [L, c["AW"]], F32, kind="ExternalInput").ap()
        self.w_out = dt("w_out", [L, c["CW"] + c["AW"], D], F32, kind="ExternalInput").ap()
        self.fnorm = dt("final_norm", [1, D], F32, kind="ExternalInput").ap()
        self.identd = dt("ident", [128, 128], F32, kind="ExternalInput").ap()
        self.cmaskd = dt("cmask", [128, NCG * 128], F32, kind="ExternalInput").ap()
        self.out = dt("out", [T, D], F32, kind="ExternalOutput").ap()
        GT = c["GT"]
        self.xr = dt("xr", [T, D], F32, kind="Internal").ap()
        self.modd = dt("modd", [L, 9 * D], F32).ap()
        self.modx = dt("modx", [L, 9 * D], F32).ap()
        self.XK = dt("XK", [self.KVW, TK], F32).ap()
        self.RK = dt("RK", [self.KVW, TK], F32).ap()
        self.XV = dt("XV", [TK, self.KVW], F32).ap()
        self.RV = dt("RV", [TK, self.KVW], F32).ap()
        self.XKI = dt("XKI", [c["IDIM"], TK], F32).ap()
        self.RKI = dt("RKI", [c["IDIM"], TK], F32).ap()
        self.XH = dt("XH", [c["CW"], TK // 128 + 1, PAD], F32).ap()
        self.RH = dt("RH", [c["CW"], TK // 128 + 1, PAD], F32).ap()
        self.act_scr = dt("act_scr", [DFF // 128, 128, GT], BF16, kind="Internal").ap()
        self.uT = dt("uT", [c["CW"], T], F32, kind="Internal").ap()
        self.yT = dt("yT", [c["CW"], T], F32, kind="Internal").ap()
        self.ycT = dt("ycT", [(c["CW"] + c["AW"]) // 128, 128, T], BF16, kind="Internal").ap()
        self.qT = dt("qT", [c["AW"], T], BF16, kind="Internal").ap()
        self.kT = dt("kT", [self.KVW, T], BF16, kind="Internal").ap()
        self.vS = dt("vS", [T, self.KVW], BF16, kind="Internal").ap()
        self.qiT = dt("qiT", [self.QIW, T], BF16, kind="Internal").ap()
        self.kiT = dt("kiT", [c["IDIM"], T], BF16, kind="Internal").ap()
        self.wiS = dt("wiS", [T, c["IH"]], F32, kind="Internal").ap()

    def f(self, o, n):
        return self.A[:, o:o + n]

    def b(self, o, n):
        return self.A[:, o:o + n // 2].bitcast(BF16)

    def phase_end(self, waits):
        P = self.P
        for s, v in waits:
            P.wait("sp", s, v)
        v = P.op("sp", lambda e: e.sem_inc(self.s_ph.h, 1))
        self.s_ph.n += 1
        for eng in ("act", "pe", "dve", "pool"):
            P.wait(eng, self.s_ph, self.s_ph.n)

    def build(self):
        c = self.c
        nc = self.nc
        with ExitStack() as stack:
            P = self.P = Prog(nc, stack)
            AWORDS = 46592
            self.A = P.sb("A", [128, AWORDS], F32)
            self.small = P.sb("small", [128, 256], F32)
            self.identf = P.sb("identf", [128, 128], F32)
            self.identb = P.sb("identb", [128, 128], BF16)
            self.cmask = P.sb("cmask_sb", [128, c["NCG"] * 128], F32)
            self.onehot = P.sb("onehot_sb", [128, c["NCG"]], F32)
            self.RG = [list(range(b * c["NCG"], (b + 1) * c["NCG"])) for b in range(c["NB"])]
            self.ones = P.sb("ones", [128, 128], F32)
            self.banks = [P.ps(f"bank{i}", [128, 512], F32) for i in range(8)]
            names = ["ph", "c", "c2", "x", "ss", "d1", "h", "tp", "ev", "w", "mm", "sg", "a", "ao", "al", "wd",
                     "p3", "xo", "e3", "e3a", "st", "g", "m1", "m2", "m3", "m4", "m5", "m6", "x1"]
            for n in names:
                setattr(self, "s_" + n, P.sem("s_" + n))
            small = self.small
            self.ss = small[:, 0:8]
            self.rs = small[:, 8:16]
            self.epsb = small[:, 16:17]
            P.op("sp", lambda e: e.dma_start(out=self.identf[:], in_=self.identd), self.s_c, dma=True)
            P.op("sp", lambda e: e.dma_start(out=self.cmask[:], in_=self.cmaskd), self.s_c, dma=True)
            P.op("sp", lambda e: e.dma_start(out=self.onehot[:], in_=self.onehotd), self.s_c, dma=True)
            P.wait("dve", self.s_c, self.s_c.n)
            P.op("dve", lambda e: e.tensor_copy(out=self.identb[:], in_=self.identf[:]))
            P.op("dve", lambda e: e.memset(self.ss, 0.0))
            P.op("dve", lambda e: e.memset(self.ones[:], 1.0))
            v = P.op("dve", lambda e: e.memset(self.epsb, EPS), self.s_c2)
            for eng in ("act", "pe", "pool", "sp"):
                P.wait(eng, self.s_c2, v)

            self.ada()
            T, GT = c["T"], c["GT"]
            for l in range(c["DEPTH"]):
                md = lambda i, l=l: self.modd[l:l + 1, i * c["D"]:(i + 1) * c["D"]]
                for g in range(T // GT):
                    self.ffn_group(l, 0, g, md, first=(l == 0))
                self.mix(l, md)
                for g in range(T // GT):
                    self.ffn_group(l, 1, g, md, first=False)
            self.final()
            P.emit()
        return nc

    def ada(self):
        c, P = self.c, self.P
        D = c["D"]
        KC = D // 128
        NCG = c["NCG"]
        W = 9 * D // NCG
        NB = W // 512
        cT = self.small[:, 32:32 + KC]
        wblk = [self.f(i * KC * 512, KC * 512).rearrange("p (a b) -> p a b", a=KC) for i in range(2)]
        o = 2 * KC * 512
        bb = [self.f(o + i * 512, 512) for i in range(2)]
        ob = [self.f(o + 1024 + i * 512, 512) for i in range(2)]
        obm = [[self.f(o + 2048 + (r * NCG + i) * 512, 512) for i in range(NCG)] for r in range(2)]
        s_ld, s_pe, s_ev, s_st, s_c = self.s_m1, self.s_m2, self.s_m3, self.s_m4, self.s_c
        v = P.op("sp", lambda e: e.dma_start(out=cT, in_=self.cin.rearrange("o (kc p) -> p (o kc)", p=128),
                                             allow_slow_non_contiguous=True), s_c, dma=True)
        P.wait("act", s_c, v)
        vs = P.op("act", lambda e: e.activation(out=cT, in_=cT, func=AF.Silu), self.s_c2)
        P.wait("pe", self.s_c2, vs)
        KG = min(8, KC)
        idx = 0
        pe_vals, ev_vals, st_vals = [], [], []
        for l in range(c["DEPTH"]):
            wv = self.ada_w[l].rearrange("(kc p) f -> p kc f", p=128)
            for blk in range(NB):
                r = idx % 2
                if idx >= 2:
                    P.wait("sp", s_pe, pe_vals[idx - 2])
                    P.wait("sp", s_ev, ev_vals[idx - 2])
                for kg in range(KC // KG):
                    P.op("sp", lambda e, r=r, kg=kg, blk=blk, wv=wv: e.dma_start(
                        out=wblk[r][:, kg * KG:(kg + 1) * KG, :], in_=wv[:, kg * KG:(kg + 1) * KG, blk * 512:(blk + 1) * 512]),
                        s_ld, dma=True)
                vld = P.op("sp", lambda e, r=r, l=l, blk=blk: e.dma_start(
                    out=bb[r][0:1, :], in_=self.ada_b[l:l + 1, blk * 512:(blk + 1) * 512]), s_ld, dma=True)
                P.wait("pe", s_ld, vld)
                if idx >= 2:
                    P.wait("pe", s_ev, ev_vals[idx - 2])
                for kc in range(KC):
                    fn = lambda e, r=r, kc=kc: e.matmul(self.banks[r][0:1, :], cT[:, kc:kc + 1], wblk[r][:, kc, :],
                                                        start=(kc == 0), stop=(kc == KC - 1))
                    if kc == KC - 1:
                        vpe = P.op("pe", fn, s_pe)
                    else:
                        P.op("pe", fn)
                pe_vals.append(vpe)
                P.wait("dve", s_pe, vpe)
                if idx >= 2:
                    P.wait("dve", s_st, st_vals[idx - 2])
                vev = P.op("dve", lambda e, r=r: e.tensor_tensor(out=ob[r][0:1, :], in0=self.banks[r][0:1, :], in1=bb[r][0:1, :],
                                                                 op=ALU.add), s_ev)
                ev_vals.append(vev)
                P.wait("dve", s_ev, vev)
                for i in range(NCG):
                    vm = P.op("dve", lambda e, r=r, i=i: e.tensor_scalar(out=obm[r][i][0:1, :], in0=ob[r][0:1, :], scalar1=self.onehot[0:1, i:i + 1], scalar2=None, op0=ALU.mult), self.s_m5)
                P.wait("act", self.s_m5, vm)
                for i in range(NCG):
                    vst = P.op("act", lambda e, r=r, l=l, blk=blk, i=i: e.dma_start(
                        out=self.modx[l:l + 1, i * W + blk * 512:i * W + (blk + 1) * 512], in_=obm[r][i][0:1, :]), s_st, dma=True)
                st_vals.append(vst)
                idx += 1
        P.wait("pool", s_st, st_vals[-1])
        vx = P.op("pool", lambda e: e.collective_compute("AllReduce", ALU.add, replica_groups=self.RG, ins=[self.modx.opt()], outs=[self.modd.opt()]), self.s_x1)
        self.phase_end([(self.s_x1, vx)])

    def norm_T(self, xsrc, NTg, g_ap, sh_ap, sc_ap, hT, o_scr, Wb, Sb):
        c, P = self.c, self.P
        D = c["D"]
        KC = D // 128
        NTT = NTg // 128
        xbuf = [self.f(o_scr + i * D, D) for i in range(2)]
        tmpb = self.f(o_scr + 2 * D, D)
        hb = [self.b(o_scr + 3 * D + i * (D // 2), D) for i in range(2)]
        pst = [self.banks[6][:].bitcast(BF16), self.banks[7][:].bitcast(BF16)]
        ss, rs, epsb = self.ss, self.rs, self.epsb
        s_c, s_c2, s_x, s_ss, s_d1, s_h, s_tp, s_ev = self.s_c, self.s_c2, self.s_x, self.s_ss, self.s_d1, self.s_h, self.s_tp, self.s_ev
        P.op("sp", lambda e: e.dma_start(out=Wb, in_=sc_ap.partition_broadcast(128)), s_c, dma=True)
        v = P.op("sp", lambda e: e.dma_start(out=Sb, in_=g_ap.partition_broadcast(128)), s_c, dma=True)
        P.wait("dve", s_c, v)
        if sh_ap is not None:
            v = P.op("dve", lambda e: e.scalar_tensor_tensor(out=Wb, in0=Wb, scalar=1.0, in1=Sb, op0=ALU.add, op1=ALU.mult), s_c2)
            P.wait("sp", s_c2, v)
            vconst = P.op("sp", lambda e: e.dma_start(out=Sb, in_=sh_ap.partition_broadcast(128)), s_c, dma=True)
        else:
            vconst = v
        vz = P.op("dve", lambda e: e.memset(ss, 0.0), s_d1)
        P.wait("act", s_d1, vz)
        h_vals, tp_done = [], {}
        ev_last = 0
        ngrp = (KC + 7) // 8
        bank_ev = [[], []]
        for tt in range(NTT):
            bsel = tt % 2
            if tt >= 2:
                P.wait("sp", s_h, h_vals[tt - 2])
            vx = P.op("sp", lambda e, tt=tt, bsel=bsel: e.dma_start(out=xbuf[bsel], in_=xsrc[tt * 128:(tt + 1) * 128, :]), s_x, dma=True)
            P.wait("act", s_x, vx)
            if tt >= 2:
                P.wait("act", s_tp, tp_done[tt - 2])
            v = P.op("act", lambda e, tt=tt, bsel=bsel: e.activation(out=hb[bsel], in_=xbuf[bsel], func=AF.Square, accum_out=ss[:, tt:tt + 1]), s_ss)
            P.wait("act", s_ss, v)
            vss = P.op("act", lambda e, tt=tt: e.activation(out=rs[:, tt:tt + 1], in_=ss[:, tt:tt + 1], func=AF.Sqrt, scale=1.0 / D, bias=epsb), s_ss)
            P.wait("dve", s_ss, vss)
            if tt == 0:
                P.wait("dve", s_c, vconst)
            v = P.op("dve", lambda e, tt=tt: e.reciprocal(out=rs[:, tt:tt + 1], in_=rs[:, tt:tt + 1]), s_d1)
            P.wait("dve", s_d1, v)
            if sh_ap is not None:
                v = P.op("dve", lambda e, tt=tt, bsel=bsel: e.scalar_tensor_tensor(out=tmpb, in0=xbuf[bsel], scalar=rs[:, tt:tt + 1], in1=Wb, op0=ALU.mult, op1=ALU.mult), s_d1)
                P.wait("dve", s_d1, v)
                vh = P.op("dve", lambda e, bsel=bsel: e.tensor_tensor(out=hb[bsel], in0=tmpb, in1=Sb, op=ALU.add), s_h)
            else:
                vh = P.op("dve", lambda e, tt=tt, bsel=bsel: e.scalar_tensor_tensor(out=tmpb, in0=xbuf[bsel], scalar=rs[:, tt:tt + 1], in1=Sb, op0=ALU.mult, op1=ALU.mult), s_h)
            h_vals.append(vh)
            if hT is None:
                P.wait("act", s_h, vh)
                vst = P.op("act", lambda e, tt=tt, off=self._fin_off: e.dma_start(out=self.out[tt * 128 + off:(tt + 1) * 128 + off, :], in_=tmpb), self.s_st, dma=True)
                P.wait("dve", self.s_st, vst)
                tp_done[tt] = 0
                continue
            P.wait("pe", s_h, vh)
            for g in range(ngrp):
                pb = (tt * ngrp + g) % 2
                if bank_ev[pb]:
                    P.wait("pe", s_ev, bank_ev[pb][-1])
                n_in = min(8, KC - g * 8)
                for i in range(n_in):
                    kc = g * 8 + i
                    fn = lambda e, pb=pb, i=i, kc=kc, bsel=bsel: e.transpose(out=pst[pb][:, i * 128:(i + 1) * 128], in_=hb[bsel][:, kc * 128:(kc + 1) * 128], identity=self.identb[:])
                    if i == n_in - 1:
                        vtp = P.op("pe", fn, s_tp)
                    else:
                        P.op("pe", fn)
                tp_done[tt] = vtp
                P.wait("act", s_tp, vtp)
                vev = P.op("act", lambda e, pb=pb, g=g, n_in=n_in, tt=tt: e.copy(
                    out=hT[:, g * 8:g * 8 + n_in, tt * 128:(tt + 1) * 128],
                    in_=pst[pb][:, 0:n_in * 128].rearrange("p (a b) -> p a b", a=n_in)), s_ev)
                bank_ev[pb].append(vev)
                ev_last = vev
        return ev_last, tp_done.get(NTT - 1, 0), h_vals[-1]

    def fm_proj(self, wsrc, jobs, hT, NTg, o_w, o_q, pre_waits, gated_func=None):
        c, P = self.c, self.P
        D = c["D"]
        KC = D // 128
        TGW = min(512, NTg)
        NTG = NTg // TGW
        wv = wsrc.rearrange("(kc p) f -> p kc f", p=128)
        wb_w = KC * 256 // 2
        wbuf = [self.b(o_w + i * wb_w, KC * 256).rearrange("p (a b) -> p a b", a=KC) for i in range(2)]
        sgbuf = [self.f(o_q + i * TGW, TGW) for i in range(2)]
        outf = [self.f(o_q + 2 * TGW + i * NTg, NTg) for i in range(2)]
        outb = [self.b(o_q + 2 * TGW + i * NTg, NTg) for i in range(2)]
        s_w, s_mm, s_sg, s_a, s_ao = self.s_w, self.s_mm, self.s_sg, self.s_a, self.s_ao
        KG = min(8, KC)
        mm_hist = getattr(self, "_mm_hist", [])
        job_mm, unit_a, ao_vals = [], [], []
        unit = 0
        for j, jb in enumerate(jobs):
            wbi = j % 2
            m = jb["m"]
            if j >= 2:
                P.wait("pool", s_mm, job_mm[j - 2])
            else:
                for s, v in pre_waits:
                    P.wait("pool", s, v)
            ops = [(0, jb["colA"])] + ([(1, jb["colB"])] if jb["colB"] is not None else [])
            for half, c0 in ops:
                for kg in range(KC // KG):
                    P.op("pool", lambda e, wbi=wbi, half=half, c0=c0, kg=kg, m=m: e.dma_start(
                        out=wbuf[wbi][:, kg * KG:(kg + 1) * KG, half * 128:half * 128 + m],
                        in_=wv[:, kg * KG:(kg + 1) * KG, c0:c0 + m]), s_w, dma=True)
            vw = s_w.n
            ao = j % 2
            for tg in range(NTG):
                slot = unit % 3
                pg, pu = self.banks[slot * 2], self.banks[slot * 2 + 1]
                P.wait("pe", s_w, vw)
                if unit == 0:
                    for s, v in pre_waits:
                        P.wait("pe", s, v)
                if unit >= 3:
                    P.wait("pe", s_a, unit_a[unit - 3])
                for half, c0 in ops:
                    pt = pg if half == 0 else pu
                    for kc in range(KC):
                        fn = lambda e, pt=pt, wbi=wbi, kc=kc, half=half, tg=tg, m=m: e.matmul(
                            pt[0:m, 0:TGW], wbuf[wbi][:, kc, half * 128:half * 128 + m],
                            hT[:, kc, tg * TGW:(tg + 1) * TGW], start=(kc == 0), stop=(kc == KC - 1))
                        if half == len(ops) - 1 and kc == KC - 1:
                            vmm = P.op("pe", fn, s_mm)
                        else:
                            P.op("pe", fn)
                odt = jb["odt"]
                dstv = (outf if odt == F32 else outb)[ao]
                if tg == 0 and j >= 2:
                    P.wait("act", s_ao, ao_vals[j - 2])
                    P.wait("dve", s_ao, ao_vals[j - 2])
                if jb["colB"] is not None:
                    sgi = unit % 2
                    P.wait("act", s_mm, vmm)
                    if unit >= 2:
                        P.wait("act", s_a, unit_a[unit - 2])
                    vsg = P.op("act", lambda e, sgi=sgi, pg=pg, m=m: e.activation(out=sgbuf[sgi][0:m, :], in_=pg[0:m, 0:TGW], func=gated_func), s_sg)
                    P.wait("dve", s_sg, vsg)
                    va = P.op("dve", lambda e, sgi=sgi, pu=pu, dstv=dstv, tg=tg, m=m: e.tensor_tensor(
                        out=dstv[0:m, tg * TGW:(tg + 1) * TGW], in0=sgbuf[sgi][0:m, :], in1=pu[0:m, 0:TGW], op=ALU.mult), s_a)
                else:
                    P.wait("act", s_mm, vmm)
                    va = P.op("act", lambda e, pg=pg, dstv=dstv, tg=tg, m=m: e.copy(
                        out=dstv[0:m, tg * TGW:(tg + 1) * TGW], in_=pg[0:m, 0:TGW]), s_a)
                unit_a.append(va)
                unit += 1
            job_mm.append(vmm)
            P.wait("sp", s_a, unit_a[-1])
            vao = P.op("sp", lambda e, jb=jb, dstv=dstv, m=m: e.dma_start(out=jb["dst"], in_=dstv[0:m, :]), s_ao, dma=True)
            ao_vals.append(vao)
        return [(s_ao, ao_vals[-1]), (s_mm, job_mm[-1]), (s_a, unit_a[-1])]

    def down_proj(self, act_src, FC, wsrc, gate_ap, res_scale, xin, xout, NTg, o_q, pre_waits):
        c, P = self.c, self.P
        D = c["D"]
        NTT = NTg // 128
        DBW = min(512, D)
        NDB = D // DBW
        FG = min(8, FC)
        NFG = FC // FG
        act_all = self.b(0, FC * NTg).rearrange("p (a b) -> p a b", a=FC)
        wd_w = FG * DBW // 2
        wdbuf = [self.b(o_q + i * wd_w, FG * DBW).rearrange("p (a b) -> p a b", a=FG) for i in range(3)]
        o3 = o_q + 3 * wd_w
        xold = [self.f(o3 + i * DBW, DBW) for i in range(4)]
        outb = [self.f(o3 + 4 * DBW + i * DBW, DBW) for i in range(3)]
        Gb = self.f(o3 + 7 * DBW, D)
        s_g, s_al, s_c2, s_wd, s_p3, s_xo, s_e3, s_e3a, s_st = self.s_g, self.s_al, self.s_c2, self.s_wd, self.s_p3, self.s_xo, self.s_e3, self.s_e3a, self.s_st
        for s, v in pre_waits:
            P.wait("sp", s, v)
        vg = P.op("sp", lambda e: e.dma_start(out=Gb, in_=gate_ap.partition_broadcast(128)), s_g, dma=True)
        ACH = min(8, FC)
        for a0 in range(0, FC, ACH):
            P.op("sp", lambda e, a0=a0: e.dma_start(out=act_all[:, a0:a0 + ACH, :], in_=act_src[a0:a0 + ACH].rearrange("a p t -> p a t")), s_al, dma=True)
        val_al = s_al.n
        P.wait("dve", s_g, vg)
        for s, v in pre_waits:
            P.wait("dve", s, v)
        vgb = P.op("dve", lambda e: e.tensor_scalar(out=Gb, in0=Gb, scalar1=float(res_scale), scalar2=None, op0=ALU.mult), s_c2)
        wd_v = wsrc.rearrange("(fc p) d -> p fc d", p=128)
        grp = 0
        p3_vals, e3_vals, xo_vals, st_vals = [], {}, {}, []
        ev_order = [(db, tt) for db in range(NDB) for tt in range(NTT)]

        def issue_xold(idx):
            db, tt = ev_order[idx]
            if idx >= 4:
                P.wait("sp", s_e3, e3_vals[ev_order[idx - 4]])
            xo_vals[(db, tt)] = P.op("sp", lambda e, idx=idx, db=db, tt=tt: e.dma_start(
                out=xold[idx % 4], in_=xin[tt * 128:(tt + 1) * 128, db * DBW:(db + 1) * DBW]), s_xo, dma=True)

        nx_issued = 0
        for db in range(NDB):
            for fg in range(NFG):
                ri = grp % 3
                if grp >= 3:
                    P.wait("pool", s_p3, p3_vals[grp - 3])
                else:
                    for s, v in pre_waits:
                        P.wait("pool", s, v)
                vwd = P.op("pool", lambda e, ri=ri, fg=fg, db=db: e.dma_start(
                    out=wdbuf[ri], in_=wd_v[:, fg * FG:(fg + 1) * FG, db * DBW:(db + 1) * DBW]), s_wd, dma=True)
                P.wait("pe", s_wd, vwd)
                if grp == 0:
                    P.wait("pe", s_al, val_al)
                for fl in range(FG):
                    fc = fg * FG + fl
                    for tt in range(NTT):
                        if fc == 0 and db >= 1:
                            P.wait("pe", s_e3a, e3_vals[("a", db - 1, tt)])
                        fn = lambda e, tt=tt, fc=fc, ri=ri, fl=fl: e.matmul(
                            self.banks[tt][:, 0:DBW], act_all[:, fc, tt * 128:(tt + 1) * 128], wdbuf[ri][:, fl, :],
                            start=(fc == 0), stop=(fc == FC - 1))
                        if fl == FG - 1 and tt == NTT - 1:
                            vp3 = P.op("pe", fn, s_p3)
                        else:
                            P.op("pe", fn)
                p3_vals.append(vp3)
                grp += 1
            for tt in range(NTT):
                idx = db * NTT + tt
                while nx_issued <= min(idx + 3, len(ev_order) - 1):
                    if nx_issued >= 4 and ev_order[nx_issued - 4] not in e3_vals:
                        break
                    issue_xold(nx_issued)
                    nx_issued += 1
                P.wait("dve", s_p3, p3_vals[-1])
                P.wait("dve", s_c2, vgb)
                P.wait("dve", s_xo, xo_vals[(db, tt)])
                oi = idx % 3
                if idx >= 3:
                    P.wait("dve", s_st, st_vals[idx - 3])
                va = P.op("dve", lambda e, tt=tt, db=db, oi=oi: e.tensor_tensor(
                    out=outb[oi], in0=self.banks[tt][:, 0:DBW], in1=Gb[:, db * DBW:(db + 1) * DBW], op=ALU.mult), s_e3a)
                e3_vals[("a", db, tt)] = va
                P.wait("dve", s_e3a, va)
                ve = P.op("dve", lambda e, oi=oi, idx=idx: e.tensor_tensor(
                    out=outb[oi], in0=outb[oi], in1=xold[idx % 4], op=ALU.add), s_e3)
                e3_vals[(db, tt)] = ve
                P.wait("act", s_e3, ve)
                vst = P.op("act", lambda e, tt=tt, db=db, oi=oi: e.dma_start(
                    out=xout[tt * 128:(tt + 1) * 128, db * DBW:(db + 1) * DBW], in_=outb[oi]), s_st, dma=True)
                st_vals.append(vst)
        return [(s_st, st_vals[-1])]

    def ffn_group(self, l, which, g, md, first):
        c, P = self.c, self.P
        D, DFF, GT = c["D"], c["DFF"], c["GT"]
        KC = D // 128
        FC = DFF // 128
        hT_w = KC * GT // 2
        R_w = max(FC * GT // 2, hT_w + 4 * D, hT_w + 2 * KC * 128)
        hT = self.b(0, KC * GT).rearrange("p (a b) -> p a b", a=KC)
        o_q = R_w
        Wb, Sb = self.f(o_q, D), self.f(o_q + D, D)
        xsrc = (self.x if (first and which == 0) else self.xr)[g * GT:(g + 1) * GT, :]
        xdst = self.xr[g * GT:(g + 1) * GT, :]
        mi = 0 if which == 0 else 6
        ev_last, tp_last, h_last = self.norm_T(xsrc, GT, self.norms[l:l + 1, (0 if which == 0 else 2), :], md(mi), md(mi + 1), hT, hT_w, Wb, Sb)
        wsrc = self.wgu[which][l]
        jobs = [dict(colA=fc * 128, colB=DFF + fc * 128, m=128, dst=self.act_scr[fc], odt=BF16) for fc in range(FC)]
        pw = self.fm_proj(wsrc, jobs, hT, GT, hT_w, o_q + 2 * D, [(self.s_ev, ev_last), (self.s_tp, tp_last), (self.s_h, h_last)], gated_func=AF.Silu)
        fin = self.down_proj(self.act_scr, FC, self.wd[which][l], md(mi + 2), 0.5, xsrc, xdst, GT, R_w, pw)
        self.phase_end(fin)

    def final(self):
        c, P = self.c, self.P
        D, GT, T = c["D"], c["GT"], c["T"]
        for g in range(T // GT):
            self._fin_off = g * GT
            Wb, Sb = self.f(6 * D, D), self.f(7 * D, D)
            self.norm_T(self.xr[g * GT:(g + 1) * GT, :], GT, self.fnorm, None, self.fnorm, None, 0, Wb, Sb)
            self.phase_end([(self.s_st, self.s_st.n)])

    def mix(self, l, md):
        c, P = self.c, self.P
        T, D, GT, CW, AW = c["T"], c["D"], c["GT"], c["CW"], c["AW"]
        KC = D // 128
        KVW, QIW, IDIM, IH = self.KVW, self.QIW, c["IDIM"], c["IH"]
        w_in = self.w_in[l]
        oq, ok, ov, oqi, oki, owi = 2 * CW, 2 * CW + AW, 2 * CW + AW + KVW, 2 * CW + AW + 2 * KVW, 2 * CW + AW + 2 * KVW + QIW, 2 * CW + AW + 2 * KVW + QIW + IDIM
        hT_w = KC * GT // 2
        for g in range(T // GT):
            t0 = g * GT
            hT = self.b(0, KC * GT).rearrange("p (a b) -> p a b", a=KC)
            o_q = max(hT_w + max(4 * D, 2 * KC * 128), (c['DFF'] // 128) * GT // 2)
            Wb, Sb = self.f(o_q, D), self.f(o_q + D, D)
            ev_last, tp_last, h_last = self.norm_T(self.xr[t0:t0 + GT, :], GT, self.norms[l:l + 1, 1, :], md(3), md(4), hT, hT_w, Wb, Sb)
            pre = [(self.s_ev, ev_last), (self.s_tp, tp_last), (self.s_h, h_last)]
            jobs = [dict(colA=CW + cc * 128, colB=cc * 128, m=128, dst=self.uT[cc * 128:(cc + 1) * 128, t0:t0 + GT], odt=F32) for cc in range(CW // 128)]
            pw = self.fm_proj(w_in, jobs, hT, GT, hT_w, o_q + 2 * D, pre, gated_func=AF.Sigmoid)
            jobs = [dict(colA=oq + i * 128, colB=None, m=128, dst=self.qT[i * 128:(i + 1) * 128, t0:t0 + GT], odt=BF16) for i in range(AW // 128)]
            jobs += [dict(colA=ok + i * 128, colB=None, m=128, dst=self.kT[i * 128:(i + 1) * 128, t0:t0 + GT], odt=BF16) for i in range(KVW // 128)]
            jobs += [dict(colA=oqi + i * 128, colB=None, m=128, dst=self.qiT[i * 128:(i + 1) * 128, t0:t0 + GT], odt=BF16) for i in range(QIW // 128)]
            jobs += [dict(colA=oki, colB=None, m=IDIM, dst=self.kiT[:, t0:t0 + GT], odt=BF16)]
            pw = self.fm_proj(w_in, jobs, hT, GT, hT_w, o_q + 2 * D, pw, gated_func=None)
            pw = self.tm_proj(w_in, ov, KVW, self.vS[t0:t0 + GT, :], BF16, hT, GT, hT_w, o_q + 2 * D, pw)
            pw = self.tm_proj(w_in, owi, IH, self.wiS[t0:t0 + GT, :], F32, hT, GT, hT_w, o_q + 2 * D, pw)
            self.phase_end(pw)
        self.exchange(l)
        self.conv(l)
        self.attn(l)
        FC = (CW + AW) // 128
        for g in range(T // GT):
            t0 = g * GT
            fin = self.down_proj(self.ycT[:, :, t0:t0 + GT], FC, self.w_out[l], md(5), 1.0, self.xr[t0:t0 + GT, :], self.xr[t0:t0 + GT, :], GT, FC * GT // 2, [])
            self.phase_end(fin)

    def tm_proj(self, wsrc, col0, ncols, dst, odt, hT, NTg, o_w, o_q, pre_waits):
        c, P = self.c, self.P
        D = c["D"]
        KC = D // 128
        NTT = NTg // 128
        wv = wsrc.rearrange("(kc p) f -> p kc f", p=128)
        wbuf = self.b(o_w, KC * ncols).rearrange("p (a b) -> p a b", a=KC)
        of = [self.f(o_q + i * 512, ncols) for i in range(2)]
        ob = [self.b(o_q + i * 512, ncols) for i in range(2)]
        s_w, s_mm, s_a, s_ao = self.s_w, self.s_mm, self.s_a, self.s_ao
        for s, v in pre_waits:
            P.wait("pool", s, v)
            P.wait("act", s, v)
        KG = min(8, KC)
        for kg in range(KC // KG):
            P.op("pool", lambda e, kg=kg: e.dma_start(out=wbuf[:, kg * KG:(kg + 1) * KG, :], in_=wv[:, kg * KG:(kg + 1) * KG, col0:col0 + ncols]), s_w, dma=True)
        vw = s_w.n
        P.wait("pe", s_w, vw)
        a_vals, ao_vals = [], []
        for tt in range(NTT):
            bk = self.banks[tt % 2]
            if tt >= 2:
                P.wait("pe", s_a, a_vals[tt - 2])
            for kc in range(KC):
                fn = lambda e, bk=bk, kc=kc, tt=tt: e.matmul(bk[:, 0:ncols], hT[:, kc, tt * 128:(tt + 1) * 128], wbuf[:, kc, :], start=(kc == 0), stop=(kc == KC - 1))
                if kc == KC - 1:
                    vmm = P.op("pe", fn, s_mm)
                else:
                    P.op("pe", fn)
            P.wait("act", s_mm, vmm)
            if tt >= 2:
                P.wait("act", s_ao, ao_vals[tt - 2])
            dv = (of if odt == F32 else ob)[tt % 2]
            va = P.op("act", lambda e, dv=dv, bk=bk: e.copy(out=dv, in_=bk[:, 0:ncols]), s_a)
            a_vals.append(va)
            P.wait("sp", s_a, va)
            vao = P.op("sp", lambda e, dv=dv, tt=tt: e.dma_start(out=dst[tt * 128:(tt + 1) * 128, :], in_=dv), s_ao, dma=True)
            ao_vals.append(vao)
        return [(s_ao, ao_vals[-1]), (s_mm, vmm), (s_a, a_vals[-1])]

    def exchange(self, l):
        c, P = self.c, self.P
        T, CW, NCG, IDIM = c["T"], c["CW"], c["NCG"], c["IDIM"]
        KVW = self.KVW
        PAD = c["CK"] - 1
        NTL = T // 128
        s_l, s_d, s_st = self.s_m1, self.s_m2, self.s_m5
        srcb = self.b(0, 2 * T)
        srcf = self.f(T, NTL * PAD + 8)
        tmp = [self.f(2 * T + 64 + i * T, T) for i in range(NCG)]
        zt = self.f(2 * T + 64 + NCG * T, PAD + 2)

        def item(m, load_fn, src_view, tmp_view, store_fn):
            P.wait("sp", s_d, s_d.n)
            vl = P.op("sp", load_fn, s_l, dma=True)
            P.wait("dve", s_l, vl)
            P.wait("dve", s_st, s_st.n)
            for i in range(NCG):
                vd = P.op("dve", lambda e, i=i: e.tensor_scalar(out=tmp_view(i), in0=src_view, scalar1=self.onehot[0:m, i:i + 1], scalar2=None, op0=ALU.mult), s_d)
            P.wait("act", s_d, vd)
            for i in range(NCG):
                P.op("act", lambda e, i=i: store_fn(e, i), s_st, dma=True)

        v0 = P.op("dve", lambda e: e.memset(zt, 0.0), s_d)
        P.wait("act", s_d, v0)
        for cc in range(CW // 128):
            P.op("act", lambda e, cc=cc: e.dma_start(out=self.XH[cc * 128:(cc + 1) * 128, 0, :], in_=zt[:, 0:PAD]), s_st, dma=True)
        for a in range(KVW // 128):
            rows = slice(a * 128, (a + 1) * 128)
            item(128, lambda e, rows=rows: e.dma_start(out=srcb[:, 0:T], in_=self.kT[rows, :]),
                 srcb[:, 0:T], lambda i: tmp[i],
                 lambda e, i, rows=rows: e.dma_start(out=self.XK[rows, :].rearrange("p (j i t) -> p j i t", i=NCG, t=128)[:, :, i, :],
                                                     in_=tmp[i].rearrange("p (j t) -> p j t", t=128)))
        item(IDIM, lambda e: e.dma_start(out=srcb[0:IDIM, 0:T], in_=self.kiT),
             srcb[0:IDIM, 0:T], lambda i: tmp[i][0:IDIM, :],
             lambda e, i: e.dma_start(out=self.XKI.rearrange("p (j i t) -> p j i t", i=NCG, t=128)[:, :, i, :],
                                      in_=tmp[i][0:IDIM, :].rearrange("p (j t) -> p j t", t=128)))
        for j in range(NTL):
            item(128, lambda e, j=j: e.dma_start(out=srcb[:, 0:KVW], in_=self.vS[j * 128:(j + 1) * 128, :]),
                 srcb[:, 0:KVW], lambda i: tmp[i][:, 0:KVW],
                 lambda e, i, j=j: e.dma_start(out=self.XV[(NCG * j + i) * 128:(NCG * j + i + 1) * 128, :], in_=tmp[i][:, 0:KVW]))
        for cc in range(CW // 128):
            rows = slice(cc * 128, (cc + 1) * 128)
            hs = srcf[:, 0:NTL * PAD].rearrange("p (j t) -> p j t", t=PAD)
            item(128, lambda e, rows=rows, hs=hs: e.dma_start(out=hs, in_=self.uT[rows, :].rearrange("p (j t) -> p j t", t=128)[:, :, 128 - PAD:128]),
                 hs, lambda i: tmp[i][:, 0:NTL * PAD].rearrange("p (j t) -> p j t", t=PAD),
                 lambda e, i, rows=rows: e.dma_start(out=self.XH[rows, 1:, :].rearrange("p (j i) t -> p j i t", i=NCG)[:, :, i, :],
                                                     in_=tmp[i][:, 0:NTL * PAD].rearrange("p (j t) -> p j t", t=PAD)))
        P.wait("pool", s_st, s_st.n)
        vx_halo = None
        for src, dst in ((self.XH, self.RH), (self.XK, self.RK), (self.XV, self.RV), (self.XKI, self.RKI)):
            R0 = src.shape[0]
            total = 4
            for d in src.shape:
                total *= d
            pieces = max(1, -(-total // (3 << 20)))
            while R0 % pieces:
                pieces += 1
            rp = R0 // pieces
            for pi in range(pieces):
                vx = P.op("pool", lambda e, src=src, dst=dst, pi=pi, rp=rp: e.collective_compute(
                    "AllReduce", ALU.add, replica_groups=self.RG, ins=[src[pi * rp:(pi + 1) * rp].opt()], outs=[dst[pi * rp:(pi + 1) * rp].opt()]), self.s_x1)
                P.wait("pool", self.s_x1, vx)
            if vx_halo is None:
                vx_halo = vx
        self.phase_end([(self.s_x1, vx_halo)])

    def conv(self, l):
        c, P = self.c, self.P
        T, CW, CK = c["T"], c["CW"], c["CK"]
        NCC = CW // 128
        PAD = CK - 1
        o = 0
        S1 = self.f(o, T); o += T
        S2 = self.f(o, T); o += T
        S3 = self.f(o, T); o += T
        NTL = T // 128
        NCG = c["NCG"]
        SEG = PAD + 128
        ubuf = self.f(o, NTL * SEG); o += NTL * SEG
        ub3 = ubuf.rearrange("p (j t) -> p j t", t=SEG)
        hc = self.f(o, NTL * NCG * PAD).rearrange("p (j i t) -> p j i t", i=NCG, t=PAD); o += NTL * NCG * PAD
        ybuf = self.f(o, T); o += T
        yb3 = ybuf.rearrange("p (j t) -> p j t", t=128)
        zbuf = self.f(o, T); o += T
        meanb = self.f(o, T); o += T
        rstdb = self.f(o, T); o += T
        zcb = self.b(o, T); o += T // 2
        cw = self.small[:, 64:64 + CK]
        cv = self.small[:, 100:104]
        s_l, s_d, s_a, s_p, s_st = self.s_m1, self.s_m2, self.s_m3, self.s_m4, self.s_m5
        NTB = (T + 511) // 512
        TBW = min(512, T)

        def chain(eng, fn):
            v = P.op(eng, fn, s_d if eng == "dve" else s_a)
            P.wait(eng, s_d if eng == "dve" else s_a, v)
            return v

        chain("dve", lambda e: e.memset(S1, 0.0))
        chain("dve", lambda e: e.memset(S2, 0.0))
        chain("dve", lambda e: e.memset(S3, 0.0))
        st_prev = 0
        for cc in range(NCC):
            rows = slice(cc * 128, (cc + 1) * 128)
            P.wait("sp", s_d, s_d.n)
            P.wait("sp", s_a, s_a.n)
            P.op("sp", lambda e, rows=rows: e.dma_start(out=ub3[:, :, PAD:SEG], in_=self.uT[rows, :].rearrange("p (j t) -> p j t", t=128)), s_l, dma=True)
            P.op("sp", lambda e, rows=rows: e.dma_start(out=hc, in_=self.RH[rows, 0:NTL * NCG, :].rearrange("p (j i) t -> p j i t", i=NCG)), s_l, dma=True)
            P.op("sp", lambda e, rows=rows: e.dma_start(out=cw, in_=self.conv_w[l][:, rows].rearrange("k c -> c k"), allow_slow_non_contiguous=True), s_l, dma=True)
            vl = P.op("sp", lambda e, rows=rows: e.dma_start(out=cv, in_=self.cvec[l][:, rows].rearrange("k c -> c k"), allow_slow_non_contiguous=True), s_l, dma=True)
            P.wait("dve", s_l, vl)
            P.wait("dve", s_st, st_prev)
            chain("dve", lambda e: e.tensor_scalar(out=ub3[:, :, 0:PAD], in0=hc[:, :, 0, :], scalar1=self.onehot[:, 0:1], scalar2=None, op0=ALU.mult))
            for i in range(1, NCG):
                chain("dve", lambda e, i=i: e.scalar_tensor_tensor(out=ub3[:, :, 0:PAD], in0=hc[:, :, i, :], scalar=self.onehot[:, i:i + 1], in1=ub3[:, :, 0:PAD], op0=ALU.mult, op1=ALU.add))
            chain("dve", lambda e: e.tensor_scalar(out=yb3, in0=ub3[:, :, 0:128], scalar1=cw[:, 0:1], scalar2=cv[:, 0:1], op0=ALU.mult, op1=ALU.add))
            for j in range(1, CK):
                chain("dve", lambda e, j=j: e.scalar_tensor_tensor(out=yb3, in0=ub3[:, :, j:j + 128], scalar=cw[:, j:j + 1], in1=yb3, op0=ALU.mult, op1=ALU.add))
            vy = s_d.n
            P.wait("sp", s_d, vy)
            st_prev = P.op("sp", lambda e, rows=rows: e.dma_start(out=self.yT[rows, :], in_=ybuf), s_st, dma=True)
            P.wait("act", s_d, vy)
            chain("act", lambda e: e.activation(out=zbuf, in_=ybuf, func=AF.Square))
            chain("dve", lambda e: e.tensor_tensor(out=S1, in0=S1, in1=ybuf, op=ALU.add))
            P.wait("dve", s_a, s_a.n)
            chain("dve", lambda e: e.tensor_tensor(out=S2, in0=S2, in1=zbuf, op=ALU.add))
        P.wait("pe", s_d, s_d.n)
        for tb in range(NTB):
            cs = slice(tb * TBW, (tb + 1) * TBW)
            P.wait("pe", s_a, s_a.n)
            v1 = P.op("pe", lambda e, cs=cs: e.matmul(self.banks[0][:, 0:TBW], self.ones[:], S1[:, cs], start=True, stop=True), s_p)
            v2 = P.op("pe", lambda e, cs=cs: e.matmul(self.banks[1][:, 0:TBW], self.ones[:], S2[:, cs], start=True, stop=True), s_p)
            P.wait("act", s_p, v2)
            chain("act", lambda e, cs=cs: e.mul(out=meanb[:, cs], in_=self.banks[0][:, 0:TBW], mul=1.0 / CW))
            chain("act", lambda e, cs=cs: e.mul(out=rstdb[:, cs], in_=self.banks[1][:, 0:TBW], mul=1.0 / CW))
        P.wait("dve", s_a, s_a.n)
        chain("dve", lambda e: e.tensor_tensor(out=zbuf, in0=meanb, in1=meanb, op=ALU.mult))
        chain("dve", lambda e: e.tensor_tensor(out=rstdb, in0=rstdb, in1=zbuf, op=ALU.subtract))
        P.wait("act", s_d, s_d.n)
        chain("act", lambda e: e.activation(out=rstdb, in_=rstdb, func=AF.Sqrt, bias=self.epsb))
        P.wait("dve", s_a, s_a.n)
        chain("dve", lambda e: e.reciprocal(out=rstdb, in_=rstdb))
        for cc in range(NCC):
            rows = slice(cc * 128, (cc + 1) * 128)
            P.wait("sp", s_d, s_d.n)
            P.wait("sp", s_a, s_a.n)
            P.wait("sp", s_st, st_prev)
            P.op("sp", lambda e, rows=rows: e.dma_start(out=ybuf, in_=self.yT[rows, :]), s_l, dma=True)
            vl = P.op("sp", lambda e, rows=rows: e.dma_start(out=cv, in_=self.cvec[l][:, rows].rearrange("k c -> c k"), allow_slow_non_contiguous=True), s_l, dma=True)
            P.wait("dve", s_l, vl)
            chain("dve", lambda e: e.tensor_tensor(out=ybuf, in0=ybuf, in1=meanb, op=ALU.subtract))
            chain("dve", lambda e: e.tensor_tensor(out=ybuf, in0=ybuf, in1=rstdb, op=ALU.mult))
            P.wait("act", s_d, s_d.n)
            chain("act", lambda e: e.activation(out=zbuf, in_=ybuf, func=AF.Silu, scale=cv[:, 1:2], bias=cv[:, 2:3]))
            chain("act", lambda e: e.activation(out=ybuf, in_=zbuf, func=AF.Square))
            P.wait("dve", s_a, s_a.n)
            chain("dve", lambda e: e.tensor_tensor(out=S3, in0=S3, in1=ybuf, op=ALU.add))
            chain("dve", lambda e: e.tensor_scalar(out=zcb, in0=zbuf, scalar1=cv[:, 3:4], scalar2=None, op0=ALU.mult))
            P.wait("sp", s_d, s_d.n)
            st_prev = P.op("sp", lambda e, cc=cc: e.dma_start(out=self.ycT[cc], in_=zcb), s_st, dma=True)
        P.wait("pe", s_d, s_d.n)
        for tb in range(NTB):
            cs = slice(tb * TBW, (tb + 1) * TBW)
            P.wait("pe", s_a, s_a.n)
            v1 = P.op("pe", lambda e, cs=cs: e.matmul(self.banks[0][:, 0:TBW], self.ones[:], S3[:, cs], start=True, stop=True), s_p)
            P.wait("act", s_p, v1)
            chain("act", lambda e, cs=cs: e.activation(out=rstdb[:, cs], in_=self.banks[0][:, 0:TBW], func=AF.Sqrt, scale=1.0 / CW, bias=self.epsb))
        P.wait("dve", s_a, s_a.n)
        chain("dve", lambda e: e.reciprocal(out=rstdb, in_=rstdb))
        for cc in range(NCC):
            P.wait("sp", s_d, s_d.n)
            P.wait("sp", s_st, st_prev)
            vl = P.op("sp", lambda e, cc=cc: e.dma_start(out=zcb, in_=self.ycT[cc]), s_l, dma=True)
            P.wait("dve", s_l, vl)
            chain("dve", lambda e: e.tensor_tensor(out=zcb, in0=zcb, in1=rstdb, op=ALU.mult))
            P.wait("sp", s_d, s_d.n)
            st_prev = P.op("sp", lambda e, cc=cc: e.dma_start(out=self.ycT[cc], in_=zcb), s_st, dma=True)
        self.phase_end([(s_st, st_prev)])

    def attn(self, l):
        c, P = self.c, self.P
        T, CW, AW, NH, NKV, IH, IDIM, TOPK = c["T"], c["CW"], c["AW"], c["NH"], c["NKV"], c["IH"], c["IDIM"], c["TOPK"]
        GRP = NH // NKV
        NCG = c["NCG"]
        TL = T
        T = self.TK
        NT = T // 128
        NTL = TL // 128
        scale = 128 ** -0.5
        o = 0
        kT_sb = self.b(o, NKV * T).rearrange("p (a b) -> p a b", a=NKV); o += NKV * T // 2
        VW = NKV * 130
        v_sb = self.b(o, NT * VW).rearrange("p (a b d) -> p a b d", a=NT, b=NKV); o += NT * VW // 2
        kiT_sb = self.b(o, T); o += T // 2
        an_b = self.f(o, AW); o += AW
        scores = self.f(o, T); o += T
        o_wk = o
        wk = self.f(o, T); o += T
        sel = self.b(o, T); o += T // 2
        pbuf = self.b(o, T); o += T // 2
        pmbuf = self.b(o, T); o += T // 2
        pT = self.b(o, NT * 128).rearrange("p (a b) -> p a b", a=NT); o += NT * 64
        yat = self.f(o, AW); o += AW
        ynb = pbuf[:, 0:AW]
        yaT = self.b(o, AW).rearrange("p (a b) -> p a b", a=AW // 128); o += AW // 2
        qT_t = self.b(o, NH * 128).rearrange("p (a b) -> p a b", a=NH); o += NH * 64
        qiT_t = self.b(o, IH * 128).rearrange("p (a b) -> p a b", a=IH); o += IH * 64
        o_dall = o
        Dall = self.b(o, IH * 128).rearrange("p (a b) -> p a b", a=IH); o += IH * 64
        abuf = [self.b(o + i * 256, 512) for i in range(3)]; o += 768
        if IH >= NT:
            pT2 = self.b(o_dall, NT * 128).rearrange("p (a b) -> p a b", a=NT)
        else:
            pT2 = self.b(o, NT * 128).rearrange("p (a b) -> p a b", a=NT); o += NT * 64
        pbs = [pbuf, self.b(o_wk, T)]
        pms = [pmbuf, self.b(o_wk + T // 2, T)]
        pTs = [pT, pT2]
        rinv2 = [self.small[:, 211:212], self.small[:, 212:213]]
        s_L, s_E, s_M, s_T, s_V, s_PV, s_Y = self.s_w, self.s_mm, self.s_sg, self.s_a, self.s_ao, self.s_al, self.s_wd
        lgb = self.banks[0:4]
        tpbs = [self.banks[4][:].bitcast(BF16), self.banks[5][:].bitcast(BF16)]
        pob = [self.banks[6], self.banks[7]]
        wi_t = self.small[:, 128:128 + IH]
        wib = self.small[:, 160:160 + IH].bitcast(BF16)
        m8 = self.small[:, 200:208]
        thr = self.small[:, 208:209]
        ssa = self.small[:, 209:210]
        ra = self.small[:, 210:211]
        rinv = self.small[:, 211:212]
        s_l, s_d, s_a, s_p, s_st, s_q = self.s_m1, self.s_m2, self.s_m3, self.s_m4, self.s_m5, self.s_m6
        banks = self.banks
        tpb = banks[6][:].bitcast(BF16)

        def chain(eng, fn):
            sm = {"dve": s_d, "act": s_a, "pe": s_p}[eng]
            v = P.op(eng, fn, sm)
            P.wait(eng, sm, v)
            return v

        for a in range(NKV):
            P.op("pool", lambda e, a=a: e.dma_start(out=kT_sb[:, a, :], in_=self.RK[a * 128:(a + 1) * 128, :]), s_l, dma=True)
        for kt0 in range(0, NT, 8):
            n = min(8, NT - kt0)
            for kv in range(NKV):
                P.op("pool", lambda e, kt0=kt0, n=n, kv=kv: e.dma_start(
                    out=v_sb[:, kt0:kt0 + n, kv, 0:128],
                    in_=self.RV[kt0 * 128:(kt0 + n) * 128, kv * 128:(kv + 1) * 128].rearrange("(a p) d -> p a d", p=128)), s_l, dma=True)
        P.op("pool", lambda e: e.dma_start(out=kiT_sb[0:IDIM, :], in_=self.RKI), s_l, dma=True)
        vl = P.op("pool", lambda e: e.dma_start(out=an_b, in_=self.anorm[l:l + 1, :].partition_broadcast(128)), s_l, dma=True)
        P.wait("dve", s_l, vl)
        chain("dve", lambda e: e.memset(v_sb[:, :, :, 128:130], 1.0))
        chain("dve", lambda e: e.memset(ssa, 0.0))
        P.wait("pe", s_l, vl)
        P.wait("pe", s_d, s_d.n)
        st_prev = 0
        for g in range(NTL):
            S = (NCG * g + NCG) * 128
            nkt = S // 128
            nkb = (S + 511) // 512
            ts = slice(g * 128, (g + 1) * 128)
            dsl = slice(NCG * g * 128, S)
            P.wait("sp", s_p, s_p.n)
            P.wait("sp", s_d, s_d.n)
            P.op("sp", lambda e, ts=ts: e.dma_start(out=qT_t, in_=self.qT[:, ts].rearrange("(h p) t -> p h t", p=128)), s_q, dma=True)
            P.op("sp", lambda e, ts=ts: e.dma_start(out=qiT_t[0:IDIM, :, :], in_=self.qiT[:, ts].rearrange("(h p) t -> p h t", p=IDIM)), s_q, dma=True)
            vq = P.op("sp", lambda e, ts=ts: e.dma_start(out=wi_t, in_=self.wiS[ts, :]), s_q, dma=True)
            P.wait("dve", s_q, vq)
            for h in range(IH):
                P.op("dve", lambda e, h=h: e.tensor_scalar(out=Dall[:, h, :], in0=self.identb[:], scalar1=wi_t[:, h:h + 1], scalar2=None, op0=ALU.mult))
            chain("dve", lambda e: e.memset(thr, 0.5 * NEG))
            P.wait("pe", s_q, vq)
            P.wait("pe", s_d, s_d.n)
            units = [(kb, h) for kb in range(nkb) for h in range(IH)]
            NU = len(units)
            a_hist = []
            sc_ev = {}
            for u in range(NU + 2):
                if u < NU:
                    kb, h = units[u]
                    w = min(512, S - kb * 512)
                    ks = slice(kb * 512, kb * 512 + w)
                    r = u % 3
                    if u >= 3:
                        P.wait("pe", s_a, a_hist[u - 3])
                    v1 = P.op("pe", lambda e, r=r, h=h, ks=ks, w=w: e.matmul(banks[r][:, 0:w], qiT_t[0:IDIM, h, :], kiT_sb[0:IDIM, ks], start=True, stop=True), s_p)
                    P.wait("act", s_p, v1)
                    va = P.op("act", lambda e, r=r, w=w: e.activation(out=abuf[r][:, 0:w], in_=banks[r][:, 0:w], func=AF.Relu), s_a)
                    a_hist.append(va)
                if u >= 2:
                    u2 = u - 2
                    kb, h = units[u2]
                    w = min(512, S - kb * 512)
                    ks = slice(kb * 512, kb * 512 + w)
                    r = u2 % 3
                    scb = banks[3 + kb % 2]
                    P.wait("pe", s_a, a_hist[u2])
                    if h == 0 and kb >= 2:
                        P.wait("pe", s_a, sc_ev[kb - 2])
                    vm2 = P.op("pe", lambda e, r=r, h=h, w=w, scb=scb: e.matmul(scb[:, 0:w], Dall[:, h, :], abuf[r][:, 0:w], start=(h == 0), stop=(h == IH - 1)), s_p)
                    if h == IH - 1:
                        P.wait("act", s_p, vm2)
                        sc_ev[kb] = P.op("act", lambda e, ks=ks, w=w, scb=scb: e.copy(out=scores[:, ks], in_=scb[:, 0:w]), s_a)
            P.wait("dve", s_a, s_a.n)
            chain("dve", lambda e, dsl=dsl: e.tensor_tensor(out=scores[:, dsl], in0=scores[:, dsl], in1=self.cmask[:], op=ALU.add))
            if S > TOPK:
                chain("dve", lambda e, S=S: e.tensor_copy(out=wk[:, 0:S], in_=scores[:, 0:S]))
                for r in range(TOPK // 8):
                    chain("dve", lambda e, S=S: e.max(out=m8, in_=wk[:, 0:S]))
                    if r < TOPK // 8 - 1:
                        chain("dve", lambda e, S=S: e.match_replace(out=wk[:, 0:S], in_to_replace=m8, in_values=wk[:, 0:S], imm_value=NEG))
                chain("dve", lambda e: e.tensor_reduce(out=thr, in_=m8, axis=mybir.AxisListType.X, op=ALU.min))
                chain("dve", lambda e: e.tensor_scalar(out=thr, in0=thr, scalar1=0.5 * NEG, scalar2=None, op0=ALU.max))
            chain("dve", lambda e, S=S: e.tensor_scalar(out=sel[:, 0:S], in0=scores[:, 0:S], scalar1=thr, scalar2=None, op0=ALU.is_ge))
            if g == 0:
                Lc, Tc = [0], [0]
                E_hist, V_hist = [], []
                M_val, T_last, PV_val, Y_val = {}, {}, {}, {}
                hbase = 0
            for step in range(NH + 1):
                if step < NH:
                    h = step
                    H = hbase + h
                    bsel = H % 2
                    kv = h // GRP
                    for kb in range(nkb):
                        w = min(512, S - kb * 512)
                        ks = slice(kb * 512, kb * 512 + w)
                        slot = Lc[0] % 4
                        if Lc[0] >= 4:
                            P.wait("pe", s_E, E_hist[Lc[0] - 4])
                        if step == 0 and kb == 0:
                            P.wait("pe", s_d, s_d.n)
                        vL = P.op("pe", lambda e, slot=slot, h=h, kv=kv, ks=ks, w=w: e.matmul(lgb[slot][:, 0:w], qT_t[:, h, :], kT_sb[:, kv, ks], start=True, stop=True), s_L)
                        P.wait("act", s_L, vL)
                        if kb == 0 and H >= 2:
                            P.wait("act", s_M, M_val[H - 2])
                        vE = P.op("act", lambda e, slot=slot, bsel=bsel, ks=ks, w=w: e.activation(out=pbs[bsel][:, ks], in_=lgb[slot][:, 0:w], func=AF.Exp, scale=scale), s_E)
                        E_hist.append(vE)
                        Lc[0] += 1
                    P.wait("dve", s_E, vE)
                    if H >= 2:
                        P.wait("dve", s_T, T_last[H - 2])
                    M_val[H] = P.op("dve", lambda e, bsel=bsel, S=S: e.tensor_tensor(out=pms[bsel][:, 0:S], in0=pbs[bsel][:, 0:S], in1=sel[:, 0:S], op=ALU.mult), s_M)
                if step >= 1:
                    h = step - 1
                    H = hbase + h
                    bsel = H % 2
                    kv = h // GRP
                    first = True
                    for k0 in range(0, nkt, 8):
                        n = min(8, nkt - k0)
                        tsl = Tc[0] % 2
                        P.wait("pe", s_M, M_val[H])
                        if Tc[0] >= 2:
                            P.wait("pe", s_V, V_hist[Tc[0] - 2])
                        for i in range(n):
                            fn = lambda e, i=i, k0=k0, tsl=tsl, bsel=bsel: e.transpose(out=tpbs[tsl][:, i * 128:(i + 1) * 128], in_=pms[bsel][:, (k0 + i) * 128:(k0 + i + 1) * 128], identity=self.identb[:])
                            if i == n - 1:
                                vT = P.op("pe", fn, s_T)
                            else:
                                P.op("pe", fn)
                        P.wait("act", s_T, vT)
                        if first and H >= 2:
                            P.wait("act", s_PV, PV_val[H - 2])
                        first = False
                        vV = P.op("act", lambda e, k0=k0, n=n, tsl=tsl, bsel=bsel: e.copy(out=pTs[bsel][:, k0:k0 + n, :], in_=tpbs[tsl][:, 0:n * 128].rearrange("p (a b) -> p a b", a=n)), s_V)
                        V_hist.append(vV)
                        Tc[0] += 1
                    T_last[H] = vT
                    P.wait("pe", s_V, vV)
                    if H >= 2:
                        P.wait("pe", s_Y, Y_val[H - 2])
                    for kt in range(nkt):
                        fn = lambda e, kt=kt, kv=kv, nkt=nkt, bsel=bsel: e.matmul(pob[bsel][:, 0:129], pTs[bsel][:, kt, :], v_sb[:, kt, kv, 0:129], start=(kt == 0), stop=(kt == nkt - 1))
                        if kt == nkt - 1:
                            vPV = P.op("pe", fn, s_PV)
                        else:
                            P.op("pe", fn)
                    PV_val[H] = vPV
                    P.wait("dve", s_PV, vPV)
                    vr = P.op("dve", lambda e, bsel=bsel: e.reciprocal(out=rinv2[bsel], in_=pob[bsel][:, 128:129]), s_Y)
                    P.wait("dve", s_Y, vr)
                    Y_val[H] = P.op("dve", lambda e, h=h, bsel=bsel: e.tensor_scalar(out=yat[:, h * 128:(h + 1) * 128], in0=pob[bsel][:, 0:128], scalar1=rinv2[bsel], scalar2=None, op0=ALU.mult), s_Y)
            hbase += NH
            P.wait("act", s_Y, Y_val[hbase - 1])
            P.wait("dve", s_Y, Y_val[hbase - 1])
            vfl = P.op("dve", lambda e: e.memset(thr, 0.5 * NEG), s_d)
            P.wait("dve", s_d, vfl)
            P.wait("pe", s_d, vfl)
            P.wait("sp", s_d, vfl)
            P.wait("act", s_d, s_d.n)
            P.wait("act", s_st, st_prev)
            chain("act", lambda e: e.activation(out=ynb, in_=yat, func=AF.Square, accum_out=ssa))
            chain("act", lambda e: e.activation(out=ra, in_=ssa, func=AF.Sqrt, scale=1.0 / AW, bias=self.epsb))
            P.wait("dve", s_a, s_a.n)
            chain("dve", lambda e: e.reciprocal(out=ra, in_=ra))
            chain("dve", lambda e: e.scalar_tensor_tensor(out=ynb, in0=yat, scalar=ra, in1=an_b, op0=ALU.mult, op1=ALU.mult))
            chain("dve", lambda e: e.memset(ssa, 0.0))
            P.wait("pe", s_d, s_d.n)
            for k0 in range(0, AW // 128, 8):
                n = min(8, AW // 128 - k0)
                P.wait("pe", s_a, s_a.n)
                for i in range(n):
                    v1 = P.op("pe", lambda e, i=i, k0=k0: e.transpose(out=tpb[:, i * 128:(i + 1) * 128], in_=ynb[:, (k0 + i) * 128:(k0 + i + 1) * 128], identity=self.identb[:]), s_p)
                P.wait("act", s_p, v1)
                chain("act", lambda e, k0=k0, n=n: e.copy(out=yaT[:, k0:k0 + n, :], in_=tpb[:, 0:n * 128].rearrange("p (a b) -> p a b", a=n)))
            P.wait("sp", s_a, s_a.n)
            st_prev = P.op("sp", lambda e, ts=ts: e.dma_start(out=self.ycT[CW // 128:, :, ts].rearrange("a p t -> p a t"), in_=yaT), s_st, dma=True)
        self.phase_end([(s_st, st_prev)])


_CACHE = {}


def _run(cfg, inputs, ncores=None):
    key = tuple(sorted(cfg.items()))
    if key not in _CACHE:
        _CACHE[key] = MK(cfg).build()
    nc = _CACHE[key]
    NCG, NB, TL, D = cfg["NCG"], cfg["NB"], cfg["T"], cfg["D"]
    NTL = TL // 128
    W = 9 * D // NCG
    ident = np.eye(128, dtype=np.float32)
    tri = np.where(np.arange(128)[None, :] <= np.arange(128)[:, None], 0.0, NEG).astype(np.float32)
    norms = np.ascontiguousarray(np.stack([inputs["ffn1_norm"], inputs["mix_norm"], inputs["ffn2_norm"]], axis=1))
    cvec = np.ascontiguousarray(np.stack([inputs["conv_b"], inputs["conv_ln_g"], inputs["conv_ln_b"], inputs["conv_out_norm"]], axis=1))
    shared = dict(norms=norms, ffn1_wgu=inputs["ffn1_wgu"], ffn2_wgu=inputs["ffn2_wgu"],
                  ffn1_wd=inputs["ffn1_wd"], ffn2_wd=inputs["ffn2_wd"], w_in=inputs["w_in"], conv_w=inputs["conv_w"], cvec=cvec,
                  attn_out_norm=inputs["attn_out_norm"], w_out=inputs["w_out"], final_norm=inputs["final_norm"].reshape(1, -1),
                  ident=ident)
    in_maps = []
    for b in range(NB):
        xb = inputs["x"][b].reshape(NTL, NCG, 128, D)
        for q in range(NCG):
            m = dict(shared)
            m["x"] = np.ascontiguousarray(xb[:, q]).reshape(TL, D)
            m["c"] = np.ascontiguousarray(inputs["c"][b:b + 1])
            m["ada_w"] = np.ascontiguousarray(inputs["ada_w"][:, :, q * W:(q + 1) * W])
            m["ada_b"] = np.ascontiguousarray(inputs["ada_b"][:, q * W:(q + 1) * W])
            oh = np.zeros((128, NCG), np.float32)
            oh[:, q] = 1.0
            m["onehot"] = oh
            cm = np.full((128, NCG, 128), NEG, np.float32)
            cm[:, :q, :] = 0.0
            cm[:, q, :] = tri
            m["cmask"] = cm.reshape(128, NCG * 128)
            in_maps.append(m)
    res = run_bass_kernel_spmd(nc, in_maps, core_ids=list(range(NB * NCG)))
    out = np.empty((NB, NTL, NCG, 128, D), np.float32)
    for b in range(NB):
        for q in range(NCG):
            out[b, :, q] = res.results[b * NCG + q]["out"].reshape(NTL, 128, D)
    return out.reshape(NB, NTL * NCG * 128, D)


def kernel(**inputs):
    inputs = {k: np.asarray(v) for k, v in inputs.items()}
    return _run(FULL, inputs)
```

```python
import numpy as np
from contextlib import ExitStack
import concourse.bass as bass
import concourse.mybir as mybir
from concourse.bass_utils import run_bass_kernel_spmd

F32 = mybir.dt.float32
BF16 = mybir.dt.bfloat16
AF = mybir.ActivationFunctionType
ALU = mybir.AluOpType
EPS = 1e-6
NEG = -1.0e30

FULL = dict(T=1024, D=4096, DFF=8192, CW=2048, AW=2048, NH=16, NKV=4, IH=32, IDIM=64, TOPK=256, CK=31,
            DEPTH=2, GT=1024, NCG=4, NB=2)


class Sem:
    def __init__(self, h):
        self.h = h
        self.n = 0


class Prog:
    def __init__(self, nc, stack):
        self.nc = nc
        self.stack = stack
        self.q = {k: [] for k in ("sp", "act", "pe", "dve", "pool")}
        self.waited = {}

    def sem(self, name):
        return Sem(self.stack.enter_context(self.nc.semaphore(name)))

    def sb(self, name, shape, dt):
        return self.stack.enter_context(self.nc.sbuf_tensor(name, shape, dt))

    def ps(self, name, shape, dt):
        return self.stack.enter_context(self.nc.psum_tensor(name, shape, dt))

    def op(self, eng, fn, sem=None, dma=False):
        if sem is not None:
            inc = 16 if dma else 1
            sem.n += inc
            self.q[eng].append(lambda e, fn=fn, h=sem.h, inc=inc: fn(e).then_inc(h, inc))
            return sem.n
        self.q[eng].append(lambda e, fn=fn: fn(e))
        return None

    def wait(self, eng, sem, val):
        if val is None or val <= 0:
            return
        key = (eng, id(sem))
        if self.waited.get(key, 0) >= val:
            return
        self.waited[key] = val
        self.q[eng].append(lambda e, h=sem.h, v=val: e.wait_ge(h, v))

    def emit(self):
        q = self.q
        with self.nc.Block() as block:
            @block.sync
            def _(e):
                for f in q["sp"]:
                    f(e)

            @block.scalar
            def _(e):
                for f in q["act"]:
                    f(e)

            @block.tensor
            def _(e):
                for f in q["pe"]:
                    f(e)

            @block.vector
            def _(e):
                for f in q["dve"]:
                    f(e)

            @block.gpsimd
            def _(e):
                for f in q["pool"]:
                    f(e)


class MK:
    def __init__(self, cfg):
        self.c = cfg
        c = cfg
        T, D, DFF = c["T"], c["D"], c["DFF"]
        self.KVW = c["NKV"] * 128
        self.QIW = c["IH"] * c["IDIM"]
        self.NIN = 2 * c["CW"] + c["AW"] + 2 * self.KVW + self.QIW + c["IDIM"] + c["IH"]
        L = c["DEPTH"]
        nc = self.nc = bass.Bass("TRN2", target_bir_lowering=False)
        dt = nc.dram_tensor
        self.x = dt("x", [T, D], F32, kind="ExternalInput").ap()
        self.cin = dt("c", [1, D], F32, kind="ExternalInput").ap()
        NCG = c["NCG"]
        TK = self.TK = T * NCG
        PAD = c["CK"] - 1
        self.ada_w = dt("ada_w", [L, D, 9 * D // NCG], F32, kind="ExternalInput").ap()
        self.ada_b = dt("ada_b", [L, 9 * D // NCG], F32, kind="ExternalInput").ap()
        self.onehotd = dt("onehot", [128, NCG], F32, kind="ExternalInput").ap()
        self.norms = dt("norms", [L, 3, D], F32, kind="ExternalInput").ap()
        self.wgu = [dt(f"ffn{i}_wgu", [L, D, 2 * DFF], F32, kind="ExternalInput").ap() for i in (1, 2)]
        self.wd = [dt(f"ffn{i}_wd", [L, DFF, D], F32, kind="ExternalInput").ap() for i in (1, 2)]
        self.w_in = dt("w_in", [L, D, self.NIN], F32, kind="ExternalInput").ap()
        self.conv_w = dt("conv_w", [L, c["CK"], c["CW"]], F32, kind="ExternalInput").ap()
        self.cvec = dt("cvec", [L, 4, c["CW"]], F32, kind="ExternalInput").ap()
        self.anorm = dt("attn_out_norm", [L, c["AW"]], F32, kind="ExternalInput").ap()
        self.w_out = dt("w_out", [L, c["CW"] + c["AW"], D], F32, kind="ExternalInput").ap()
        self.fnorm = dt("final_norm", [1, D], F32, kind="ExternalInput").ap()
        self.identd = dt("ident", [128, 128], F32, kind="ExternalInput").ap()
        self.cmaskd = dt("cmask", [128, NCG * 128], F32, kind="ExternalInput").ap()
        self.out = dt("out", [T, D], F32, kind="ExternalOutput").ap()
        GT = c["GT"]
        self.xr = dt("xr", [T, D], F32, kind="Internal").ap()
        self.modd = dt("modd", [L, 9 * D], F32).ap()
        self.modx = dt("modx", [L, 9 * D], F32).ap()
        self.XK = dt("XK", [self.KVW, TK], F32).ap()
        self.RK = dt("RK", [self.KVW, TK], F32).ap()
        self.XV = dt("XV", [TK, self.KVW], F32).ap()
        self.RV = dt("RV", [TK, self.KVW], F32).ap()
        self.XKI = dt("XKI", [c["IDIM"], TK], F32).ap()
        self.RKI = dt("RKI", [c["IDIM"], TK], F32).ap()
        self.XH = dt("XH", [c["CW"], TK // 128 + 1, PAD], F32).ap()
        self.RH = dt("RH", [c["CW"], TK // 128 + 1, PAD], F32).ap()
        self.act_scr = dt("act_scr", [DFF // 128, 128, GT], BF16, kind="Internal").ap()
        self.uT = dt("uT", [c["CW"], T], F32, kind="Internal").ap()
        self.yT = dt("yT", [c["CW"], T], F32, kind="Internal").ap()
        self.ycT = dt("ycT", [(c["CW"] + c["AW"]) // 128, 128, T], BF16, kind="Internal").ap()
        self.qT = dt("qT", [c["AW"], T], BF16, kind="Internal").ap()
        self.kT = dt("kT", [self.KVW, T], BF16, kind="Internal").ap()
        self.vS = dt("vS", [T, self.KVW], BF16, kind="Internal").ap()
        self.qiT = dt("qiT", [self.QIW, T], BF16, kind="Internal").ap()
        self.kiT = dt("kiT", [c["IDIM"], T], BF16, kind="Internal").ap()
        self.wiS = dt("wiS", [T, c["IH"]], F32, kind="Internal").ap()

    def f(self, o, n):
        return self.A[:, o:o + n]

    def b(self, o, n):
        return self.A[:, o:o + n // 2].bitcast(BF16)

    def phase_end(self, waits):
        P = self.P
        for s, v in waits:
            P.wait("sp", s, v)
        v = P.op("sp", lambda e: e.sem_inc(self.s_ph.h, 1))
        self.s_ph.n += 1
        for eng in ("act", "pe", "dve", "pool"):
            P.wait(eng, self.s_ph, self.s_ph.n)

    def build(self):
        c = self.c
        nc = self.nc
        with ExitStack() as stack:
            P = self.P = Prog(nc, stack)
            AWORDS = 46592
            self.A = P.sb("A", [128, AWORDS], F32)
            self.small = P.sb("small", [128, 256], F32)
            self.identf = P.sb("identf", [128, 128], F32)
            self.identb = P.sb("identb", [128, 128], BF16)
            self.cmask = P.sb("cmask_sb", [128, c["NCG"] * 128], F32)
            self.onehot = P.sb("onehot_sb", [128, c["NCG"]], F32)
            self.RG = [list(range(b * c["NCG"], (b + 1) * c["NCG"])) for b in range(c["NB"])]
            self.ones = P.sb("ones", [128, 128], F32)
            self.banks = [P.ps(f"bank{i}", [128, 512], F32) for i in range(8)]
            names = ["ph", "c", "c2", "x", "ss", "d1", "h", "tp", "ev", "w", "mm", "sg", "a", "ao", "al", "wd",
                     "p3", "xo", "e3", "e3a", "st", "g", "m1", "m2", "m3", "m4", "m5", "m6", "x1"]
            for n in names:
                setattr(self, "s_" + n, P.sem("s_" + n))
            small = self.small
            self.ss = small[:, 0:8]
            self.rs = small[:, 8:16]
            self.epsb = small[:, 16:17]
            P.op("sp", lambda e: e.dma_start(out=self.identf[:], in_=self.identd), self.s_c, dma=True)
            P.op("sp", lambda e: e.dma_start(out=self.cmask[:], in_=self.cmaskd), self.s_c, dma=True)
            P.op("sp", lambda e: e.dma_start(out=self.onehot[:], in_=self.onehotd), self.s_c, dma=True)
            P.wait("dve", self.s_c, self.s_c.n)
            P.op("dve", lambda e: e.tensor_copy(out=self.identb[:], in_=self.identf[:]))
            P.op("dve", lambda e: e.memset(self.ss, 0.0))
            P.op("dve", lambda e: e.memset(self.ones[:], 1.0))
            v = P.op("dve", lambda e: e.memset(self.epsb, EPS), self.s_c2)
            for eng in ("act", "pe", "pool", "sp"):
                P.wait(eng, self.s_c2, v)

            self.ada()
            T, GT = c["T"], c["GT"]
            for l in range(c["DEPTH"]):
                md = lambda i, l=l: self.modd[l:l + 1, i * c["D"]:(i + 1) * c["D"]]
                for g in range(T // GT):
                    self.ffn_group(l, 0, g, md, first=(l == 0))
                self.mix(l, md)
                for g in range(T // GT):
                    self.ffn_group(l, 1, g, md, first=False)
            self.final()
            P.emit()
        return nc

    def ada(self):
        c, P = self.c, self.P
        D = c["D"]
        KC = D // 128
        NCG = c["NCG"]
        W = 9 * D // NCG
        NB = W // 512
        cT = self.small[:, 32:32 + KC]
        wblk = [self.f(i * KC * 512, KC * 512).rearrange("p (a b) -> p a b", a=KC) for i in range(2)]
        o = 2 * KC * 512
        bb = [self.f(o + i * 512, 512) for i in range(2)]
        ob = [self.f(o + 1024 + i * 512, 512) for i in range(2)]
        obm = [[self.f(o + 2048 + (r * NCG + i) * 512, 512) for i in range(NCG)] for r in range(2)]
        s_ld, s_pe, s_ev, s_st, s_c = self.s_m1, self.s_m2, self.s_m3, self.s_m4, self.s_c
        v = P.op("sp", lambda e: e.dma_start(out=cT, in_=self.cin.rearrange("o (kc p) -> p (o kc)", p=128),
                                             allow_slow_non_contiguous=True), s_c, dma=True)
        P.wait("act", s_c, v)
        vs = P.op("act", lambda e: e.activation(out=cT, in_=cT, func=AF.Silu), self.s_c2)
        P.wait("pe", self.s_c2, vs)
        KG = min(8, KC)
        idx = 0
        pe_vals, ev_vals, st_vals = [], [], []
        for l in range(c["DEPTH"]):
            wv = self.ada_w[l].rearrange("(kc p) f -> p kc f", p=128)
            for blk in range(NB):
                r = idx % 2
                if idx >= 2:
                    P.wait("sp", s_pe, pe_vals[idx - 2])
                    P.wait("sp", s_ev, ev_vals[idx - 2])
                for kg in range(KC // KG):
                    P.op("sp", lambda e, r=r, kg=kg, blk=blk, wv=wv: e.dma_start(
                        out=wblk[r][:, kg * KG:(kg + 1) * KG, :], in_=wv[:, kg * KG:(kg + 1) * KG, blk * 512:(blk + 1) * 512]),
                        s_ld, dma=True)
                vld = P.op("sp", lambda e, r=r, l=l, blk=blk: e.dma_start(
                    out=bb[r][0:1, :], in_=self.ada_b[l:l + 1, blk * 512:(blk + 1) * 512]), s_ld, dma=True)
                P.wait("pe", s_ld, vld)
                if idx >= 2:
                    P.wait("pe", s_ev, ev_vals[idx - 2])
                for kc in range(KC):
                    fn = lambda e, r=r, kc=kc: e.matmul(self.banks[r][0:1, :], cT[:, kc:kc + 1], wblk[r][:, kc, :],
                                                        start=(kc == 0), stop=(kc == KC - 1))
                    if kc == KC - 1:
                        vpe = P.op("pe", fn, s_pe)
                    else:
                        P.op("pe", fn)
                pe_vals.append(vpe)
                P.wait("dve", s_pe, vpe)
                if idx >= 2:
                    P.wait("dve", s_st, st_vals[idx - 2])
                vev = P.op("dve", lambda e, r=r: e.tensor_tensor(out=ob[r][0:1, :], in0=self.banks[r][0:1, :], in1=bb[r][0:1, :],
                                                                 op=ALU.add), s_ev)
                ev_vals.append(vev)
                P.wait("dve", s_ev, vev)
                for i in range(NCG):
                    vm = P.op("dve", lambda e, r=r, i=i: e.tensor_scalar(out=obm[r][i][0:1, :], in0=ob[r][0:1, :], scalar1=self.onehot[0:1, i:i + 1], scalar2=None, op0=ALU.mult), self.s_m5)
                P.wait("act", self.s_m5, vm)
                for i in range(NCG):
                    vst = P.op("act", lambda e, r=r, l=l, blk=blk, i=i: e.dma_start(
                        out=self.modx[l:l + 1, i * W + blk * 512:i * W + (blk + 1) * 512], in_=obm[r][i][0:1, :]), s_st, dma=True)
                st_vals.append(vst)
                idx += 1
        P.wait("pool", s_st, st_vals[-1])
        vx = P.op("pool", lambda e: e.collective_compute("AllReduce", ALU.add, replica_groups=self.RG, ins=[self.modx.opt()], outs=[self.modd.opt()]), self.s_x1)
        self.phase_end([(self.s_x1, vx)])

    def norm_T(self, xsrc, NTg, g_ap, sh_ap, sc_ap, hT, o_scr, Wb, Sb):
        c, P = self.c, self.P
        D = c["D"]
        KC = D // 128
        NTT = NTg // 128
        xbuf = [self.f(o_scr + i * D, D) for i in range(2)]
        tmpb = self.f(o_scr + 2 * D, D)
        hb = [self.b(o_scr + 3 * D + i * (D // 2), D) for i in range(2)]
        pst = [self.banks[6][:].bitcast(BF16), self.banks[7][:].bitcast(BF16)]
        ss, rs, epsb = self.ss, self.rs, self.epsb
        s_c, s_c2, s_x, s_ss, s_d1, s_h, s_tp, s_ev = self.s_c, self.s_c2, self.s_x, self.s_ss, self.s_d1, self.s_h, self.s_tp, self.s_ev
        P.op("sp", lambda e: e.dma_start(out=Wb, in_=sc_ap.partition_broadcast(128)), s_c, dma=True)
        v = P.op("sp", lambda e: e.dma_start(out=Sb, in_=g_ap.partition_broadcast(128)), s_c, dma=True)
        P.wait("dve", s_c, v)
        if sh_ap is not None:
            v = P.op("dve", lambda e: e.scalar_tensor_tensor(out=Wb, in0=Wb, scalar=1.0, in1=Sb, op0=ALU.add, op1=ALU.mult), s_c2)
            P.wait("sp", s_c2, v)
            vconst = P.op("sp", lambda e: e.dma_start(out=Sb, in_=sh_ap.partition_broadcast(128)), s_c, dma=True)
        else:
            vconst = v
        vz = P.op("dve", lambda e: e.memset(ss, 0.0), s_d1)
        P.wait("act", s_d1, vz)
        h_vals, tp_done = [], {}
        ev_last = 0
        ngrp = (KC + 7) // 8
        bank_ev = [[], []]
        for tt in range(NTT):
            bsel = tt % 2
            if tt >= 2:
                P.wait("sp", s_h, h_vals[tt - 2])
            vx = P.op("sp", lambda e, tt=tt, bsel=bsel: e.dma_start(out=xbuf[bsel], in_=xsrc[tt * 128:(tt + 1) * 128, :]), s_x, dma=True)
            P.wait("act", s_x, vx)
            if tt >= 2:
                P.wait("act", s_tp, tp_done[tt - 2])
            v = P.op("act", lambda e, tt=tt, bsel=bsel: e.activation(out=hb[bsel], in_=xbuf[bsel], func=AF.Square, accum_out=ss[:, tt:tt + 1]), s_ss)
            P.wait("act", s_ss, v)
            vss = P.op("act", lambda e, tt=tt: e.activation(out=rs[:, tt:tt + 1], in_=ss[:, tt:tt + 1], func=AF.Sqrt, scale=1.0 / D, bias=epsb), s_ss)
            P.wait("dve", s_ss, vss)
            if tt == 0:
                P.wait("dve", s_c, vconst)
            v = P.op("dve", lambda e, tt=tt: e.reciprocal(out=rs[:, tt:tt + 1], in_=rs[:, tt:tt + 1]), s_d1)
            P.wait("dve", s_d1, v)
            if sh_ap is not None:
                v = P.op("dve", lambda e, tt=tt, bsel=bsel: e.scalar_tensor_tensor(out=tmpb, in0=xbuf[bsel], scalar=rs[:, tt:tt + 1], in1=Wb, op0=ALU.mult, op1=ALU.mult), s_d1)
                P.wait("dve", s_d1, v)
                vh = P.op("dve", lambda e, bsel=bsel: e.tensor_tensor(out=hb[bsel], in0=tmpb, in1=Sb, op=ALU.add), s_h)
            else:
                vh = P.op("dve", lambda e, tt=tt, bsel=bsel: e.scalar_tensor_tensor(out=tmpb, in0=xbuf[bsel], scalar=rs[:, tt:tt + 1], in1=Sb, op0=ALU.mult, op1=ALU.mult), s_h)
            h_vals.append(vh)
            if hT is None:
                P.wait("act", s_h, vh)
                vst = P.op("act", lambda e, tt=tt, off=self._fin_off: e.dma_start(out=self.out[tt * 128 + off:(tt + 1) * 128 + off, :], in_=tmpb), self.s_st, dma=True)
                P.wait("dve", self.s_st, vst)
                tp_done[tt] = 0
                continue
            P.wait("pe", s_h, vh)
            for g in range(ngrp):
                pb = (tt * ngrp + g) % 2
                if bank_ev[pb]:
                    P.wait("pe", s_ev, bank_ev[pb][-1])
                n_in = min(8, KC - g * 8)
                for i in range(n_in):
                    kc = g * 8 + i
                    fn = lambda e, pb=pb, i=i, kc=kc, bsel=bsel: e.transpose(out=pst[pb][:, i * 128:(i + 1) * 128], in_=hb[bsel][:, kc * 128:(kc + 1) * 128], identity=self.identb[:])
                    if i == n_in - 1:
                        vtp = P.op("pe", fn, s_tp)
                    else:
                        P.op("pe", fn)
                tp_done[tt] = vtp
                P.wait("act", s_tp, vtp)
                vev = P.op("act", lambda e, pb=pb, g=g, n_in=n_in, tt=tt: e.copy(
                    out=hT[:, g * 8:g * 8 + n_in, tt * 128:(tt + 1) * 128],
                    in_=pst[pb][:, 0:n_in * 128].rearrange("p (a b) -> p a b", a=n_in)), s_ev)
                bank_ev[pb].append(vev)
                ev_last = vev
        return ev_last, tp_done.get(NTT - 1, 0), h_vals[-1]

    def fm_proj(self, wsrc, jobs, hT, NTg, o_w, o_q, pre_waits, gated_func=None):
        c, P = self.c, self.P
        D = c["D"]
        KC = D // 128
        TGW = min(512, NTg)
        NTG = NTg // TGW
        wv = wsrc.rearrange("(kc p) f -> p kc f", p=128)
        wb_w = KC * 256 // 2
        wbuf = [self.b(o_w + i * wb_w, KC * 256).rearrange("p (a b) -> p a b", a=KC) for i in range(2)]
        sgbuf = [self.f(o_q + i * TGW, TGW) for i in range(2)]
        outf = [self.f(o_q + 2 * TGW + i * NTg, NTg) for i in range(2)]
        outb = [self.b(o_q + 2 * TGW + i * NTg, NTg) for i in range(2)]
        s_w, s_mm, s_sg, s_a, s_ao = self.s_w, self.s_mm, self.s_sg, self.s_a, self.s_ao
        KG = min(8, KC)
        mm_hist = getattr(self, "_mm_hist", [])
        job_mm, unit_a, ao_vals = [], [], []
        unit = 0
        for j, jb in enumerate(jobs):
            wbi = j % 2
            m = jb["m"]
            if j >= 2:
                P.wait("pool", s_mm, job_mm[j - 2])
            else:
                for s, v in pre_waits:
                    P.wait("pool", s, v)
            ops = [(0, jb["colA"])] + ([(1, jb["colB"])] if jb["colB"] is not None else [])
            for half, c0 in ops:
                for kg in range(KC // KG):
                    P.op("pool", lambda e, wbi=wbi, half=half, c0=c0, kg=kg, m=m: e.dma_start(
                        out=wbuf[wbi][:, kg * KG:(kg + 1) * KG, half * 128:half * 128 + m],
                        in_=wv[:, kg * KG:(kg + 1) * KG, c0:c0 + m]), s_w, dma=True)
            vw = s_w.n
            ao = j % 2
            for tg in range(NTG):
                slot = unit % 3
                pg, pu = self.banks[slot * 2], self.banks[slot * 2 + 1]
                P.wait("pe", s_w, vw)
                if unit == 0:
                    for s, v in pre_waits:
                        P.wait("pe", s, v)
                if unit >= 3:
                    P.wait("pe", s_a, unit_a[unit - 3])
                for half, c0 in ops:
                    pt = pg if half == 0 else pu
                    for kc in range(KC):
                        fn = lambda e, pt=pt, wbi=wbi, kc=kc, half=half, tg=tg, m=m: e.matmul(
                            pt[0:m, 0:TGW], wbuf[wbi][:, kc, half * 128:half * 128 + m],
                            hT[:, kc, tg * TGW:(tg + 1) * TGW], start=(kc == 0), stop=(kc == KC - 1))
                        if half == len(ops) - 1 and kc == KC - 1:
                            vmm = P.op("pe", fn, s_mm)
                        else:
                            P.op("pe", fn)
                odt = jb["odt"]
                dstv = (outf if odt == F32 else outb)[ao]
                if tg == 0 and j >= 2:
                    P.wait("act", s_ao, ao_vals[j - 2])
                    P.wait("dve", s_ao, ao_vals[j - 2])
                if jb["colB"] is not None:
                    sgi = unit % 2
                    P.wait("act", s_mm, vmm)
                    if unit >= 2:
                        P.wait("act", s_a, unit_a[unit - 2])
                    vsg = P.op("act", lambda e, sgi=sgi, pg=pg, m=m: e.activation(out=sgbuf[sgi][0:m, :], in_=pg[0:m, 0:TGW], func=gated_func), s_sg)
                    P.wait("dve", s_sg, vsg)
                    va = P.op("dve", lambda e, sgi=sgi, pu=pu, dstv=dstv, tg=tg, m=m: e.tensor_tensor(
                        out=dstv[0:m, tg * TGW:(tg + 1) * TGW], in0=sgbuf[sgi][0:m, :], in1=pu[0:m, 0:TGW], op=ALU.mult), s_a)
                else:
                    P.wait("act", s_mm, vmm)
                    va = P.op("act", lambda e, pg=pg, dstv=dstv, tg=tg, m=m: e.copy(
                        out=dstv[0:m, tg * TGW:(tg + 1) * TGW], in_=pg[0:m, 0:TGW]), s_a)
                unit_a.append(va)
                unit += 1
            job_mm.append(vmm)
            P.wait("sp", s_a, unit_a[-1])
            vao = P.op("sp", lambda e, jb=jb, dstv=dstv, m=m: e.dma_start(out=jb["dst"], in_=dstv[0:m, :]), s_ao, dma=True)
            ao_vals.append(vao)
        return [(s_ao, ao_vals[-1]), (s_mm, job_mm[-1]), (s_a, unit_a[-1])]

    def down_proj(self, act_src, FC, wsrc, gate_ap, res_scale, xin, xout, NTg, o_q, pre_waits):
        c, P = self.c, self.P
        D = c["D"]
        NTT = NTg // 128
        DBW = min(512, D)
        NDB = D // DBW
        FG = min(8, FC)
        NFG = FC // FG
        act_all = self.b(0, FC * NTg).rearrange("p (a b) -> p a b", a=FC)
        wd_w = FG * DBW // 2
        wdbuf = [self.b(o_q + i * wd_w, FG * DBW).rearrange("p (a b) -> p a b", a=FG) for i in range(3)]
        o3 = o_q + 3 * wd_w
        xold = [self.f(o3 + i * DBW, DBW) for i in range(4)]
        outb = [self.f(o3 + 4 * DBW + i * DBW, DBW) for i in range(3)]
        Gb = self.f(o3 + 7 * DBW, D)
        s_g, s_al, s_c2, s_wd, s_p3, s_xo, s_e3, s_e3a, s_st = self.s_g, self.s_al, self.s_c2, self.s_wd, self.s_p3, self.s_xo, self.s_e3, self.s_e3a, self.s_st
        for s, v in pre_waits:
            P.wait("sp", s, v)
        vg = P.op("sp", lambda e: e.dma_start(out=Gb, in_=gate_ap.partition_broadcast(128)), s_g, dma=True)
        ACH = min(8, FC)
        for a0 in range(0, FC, ACH):
            P.op("sp", lambda e, a0=a0: e.dma_start(out=act_all[:, a0:a0 + ACH, :], in_=act_src[a0:a0 + ACH].rearrange("a p t -> p a t")), s_al, dma=True)
        val_al = s_al.n
        P.wait("dve", s_g, vg)
        for s, v in pre_waits:
            P.wait("dve", s, v)
        vgb = P.op("dve", lambda e: e.tensor_scalar(out=Gb, in0=Gb, scalar1=float(res_scale), scalar2=None, op0=ALU.mult), s_c2)
        wd_v = wsrc.rearrange("(fc p) d -> p fc d", p=128)
        grp = 0
        p3_vals, e3_vals, xo_vals, st_vals = [], {}, {}, []
        ev_order = [(db, tt) for db in range(NDB) for tt in range(NTT)]

        def issue_xold(idx):
            db, tt = ev_order[idx]
            if idx >= 4:
                P.wait("sp", s_e3, e3_vals[ev_order[idx - 4]])
            xo_vals[(db, tt)] = P.op("sp", lambda e, idx=idx, db=db, tt=tt: e.dma_start(
                out=xold[idx % 4], in_=xin[tt * 128:(tt + 1) * 128, db * DBW:(db + 1) * DBW]), s_xo, dma=True)

        nx_issued = 0
        for db in range(NDB):
            for fg in range(NFG):
                ri = grp % 3
                if grp >= 3:
                    P.wait("pool", s_p3, p3_vals[grp - 3])
                else:
                    for s, v in pre_waits:
                        P.wait("pool", s, v)
                vwd = P.op("pool", lambda e, ri=ri, fg=fg, db=db: e.dma_start(
                    out=wdbuf[ri], in_=wd_v[:, fg * FG:(fg + 1) * FG, db * DBW:(db + 1) * DBW]), s_wd, dma=True)
                P.wait("pe", s_wd, vwd)
                if grp == 0:
                    P.wait("pe", s_al, val_al)
                edge = (fg == 0 and NFG > 1)
                order = ([(fl, tt) for tt in range(NTT) for fl in range(FG)] if edge
                         else [(fl, tt) for fl in range(FG) for tt in range(NTT)])
                if fg == NFG - 1:
                    p3_tt = {}
                for oi_, (fl, tt) in enumerate(order):
                    fc = fg * FG + fl
                    if fc == 0 and db >= 1:
                        P.wait("pe", s_e3a, e3_vals[("a", db - 1, tt)])
                    fn = lambda e, tt=tt, fc=fc, ri=ri, fl=fl: e.matmul(
                        self.banks[tt][:, 0:DBW], act_all[:, fc, tt * 128:(tt + 1) * 128], wdbuf[ri][:, fl, :],
                        start=(fc == 0), stop=(fc == FC - 1))
                    last = (oi_ == len(order) - 1)
                    if fg == NFG - 1 and fl == FG - 1:
                        vp3 = P.op("pe", fn, s_p3)
                        p3_tt[tt] = vp3
                    elif last:
                        vp3 = P.op("pe", fn, s_p3)
                    else:
                        P.op("pe", fn)
                p3_vals.append(vp3)
                grp += 1
            for tt in range(NTT):
                idx = db * NTT + tt
                while nx_issued <= min(idx + 3, len(ev_order) - 1):
                    if nx_issued >= 4 and ev_order[nx_issued - 4] not in e3_vals:
                        break
                    issue_xold(nx_issued)
                    nx_issued += 1
                P.wait("dve", s_p3, p3_tt[tt])
                P.wait("dve", s_c2, vgb)
                P.wait("dve", s_xo, xo_vals[(db, tt)])
                oi = idx % 3
                if idx >= 3:
                    P.wait("dve", s_st, st_vals[idx - 3])
                va = P.op("dve", lambda e, tt=tt, db=db, oi=oi: e.tensor_tensor(
                    out=outb[oi], in0=self.banks[tt][:, 0:DBW], in1=Gb[:, db * DBW:(db + 1) * DBW], op=ALU.mult), s_e3a)
                e3_vals[("a", db, tt)] = va
                P.wait("dve", s_e3a, va)
                ve = P.op("dve", lambda e, oi=oi, idx=idx: e.tensor_tensor(
                    out=outb[oi], in0=outb[oi], in1=xold[idx % 4], op=ALU.add), s_e3)
                e3_vals[(db, tt)] = ve
                P.wait("act", s_e3, ve)
                vst = P.op("act", lambda e, tt=tt, db=db, oi=oi: e.dma_start(
                    out=xout[tt * 128:(tt + 1) * 128, db * DBW:(db + 1) * DBW], in_=outb[oi]), s_st, dma=True)
                st_vals.append(vst)
        return [(s_st, st_vals[-1])]

    def ffn_group(self, l, which, g, md, first):
        c, P = self.c, self.P
        D, DFF, GT = c["D"], c["DFF"], c["GT"]
        KC = D // 128
        FC = DFF // 128
        hT_w = KC * GT // 2
        R_w = max(FC * GT // 2, hT_w + 4 * D, hT_w + 2 * KC * 128)
        hT = self.b(0, KC * GT).rearrange("p (a b) -> p a b", a=KC)
        o_q = R_w
        Wb, Sb = self.f(o_q, D), self.f(o_q + D, D)
        xsrc = (self.x if (first and which == 0) else self.xr)[g * GT:(g + 1) * GT, :]
        xdst = self.xr[g * GT:(g + 1) * GT, :]
        mi = 0 if which == 0 else 6
        ev_last, tp_last, h_last = self.norm_T(xsrc, GT, self.norms[l:l + 1, (0 if which == 0 else 2), :], md(mi), md(mi + 1), hT, hT_w, Wb, Sb)
        wsrc = self.wgu[which][l]
        jobs = [dict(colA=fc * 128, colB=DFF + fc * 128, m=128, dst=self.act_scr[fc], odt=BF16) for fc in range(FC)]
        pw = self.fm_proj(wsrc, jobs, hT, GT, hT_w, o_q + 2 * D, [(self.s_ev, ev_last), (self.s_tp, tp_last), (self.s_h, h_last)], gated_func=AF.Silu)
        fin = self.down_proj(self.act_scr, FC, self.wd[which][l], md(mi + 2), 0.5, xsrc, xdst, GT, R_w, pw)
        self.phase_end(fin)

    def final(self):
        c, P = self.c, self.P
        D, GT, T = c["D"], c["GT"], c["T"]
        for g in range(T // GT):
            self._fin_off = g * GT
            Wb, Sb = self.f(6 * D, D), self.f(7 * D, D)
            self.norm_T(self.xr[g * GT:(g + 1) * GT, :], GT, self.fnorm, None, self.fnorm, None, 0, Wb, Sb)
            self.phase_end([(self.s_st, self.s_st.n)])

    def mix(self, l, md):
        c, P = self.c, self.P
        T, D, GT, CW, AW = c["T"], c["D"], c["GT"], c["CW"], c["AW"]
        KC = D // 128
        KVW, QIW, IDIM, IH = self.KVW, self.QIW, c["IDIM"], c["IH"]
        w_in = self.w_in[l]
        oq, ok, ov, oqi, oki, owi = 2 * CW, 2 * CW + AW, 2 * CW + AW + KVW, 2 * CW + AW + 2 * KVW, 2 * CW + AW + 2 * KVW + QIW, 2 * CW + AW + 2 * KVW + QIW + IDIM
        hT_w = KC * GT // 2
        for g in range(T // GT):
            t0 = g * GT
            hT = self.b(0, KC * GT).rearrange("p (a b) -> p a b", a=KC)
            o_q = max(hT_w + max(4 * D, 2 * KC * 128), (c['DFF'] // 128) * GT // 2)
            Wb, Sb = self.f(o_q, D), self.f(o_q + D, D)
            ev_last, tp_last, h_last = self.norm_T(self.xr[t0:t0 + GT, :], GT, self.norms[l:l + 1, 1, :], md(3), md(4), hT, hT_w, Wb, Sb)
            pre = [(self.s_ev, ev_last), (self.s_tp, tp_last), (self.s_h, h_last)]
            jobs = [dict(colA=CW + cc * 128, colB=cc * 128, m=128, dst=self.uT[cc * 128:(cc + 1) * 128, t0:t0 + GT], odt=F32) for cc in range(CW // 128)]
            pw = self.fm_proj(w_in, jobs, hT, GT, hT_w, o_q + 2 * D, pre, gated_func=AF.Sigmoid)
            jobs = [dict(colA=oq + i * 128, colB=None, m=128, dst=self.qT[i * 128:(i + 1) * 128, t0:t0 + GT], odt=BF16) for i in range(AW // 128)]
            jobs += [dict(colA=ok + i * 128, colB=None, m=128, dst=self.kT[i * 128:(i + 1) * 128, t0:t0 + GT], odt=BF16) for i in range(KVW // 128)]
            jobs += [dict(colA=oqi + i * 128, colB=None, m=128, dst=self.qiT[i * 128:(i + 1) * 128, t0:t0 + GT], odt=BF16) for i in range(QIW // 128)]
            jobs += [dict(colA=oki, colB=None, m=IDIM, dst=self.kiT[:, t0:t0 + GT], odt=BF16)]
            pw = self.fm_proj(w_in, jobs, hT, GT, hT_w, o_q + 2 * D, pw, gated_func=None)
            pw = self.tm_proj(w_in, ov, KVW, self.vS[t0:t0 + GT, :], BF16, hT, GT, hT_w, o_q + 2 * D, pw)
            pw = self.tm_proj(w_in, owi, IH, self.wiS[t0:t0 + GT, :], F32, hT, GT, hT_w, o_q + 2 * D, pw)
            self.phase_end(pw)
        self.exchange(l)
        self.conv(l)
        self.attn(l)
        FC = (CW + AW) // 128
        for g in range(T // GT):
            t0 = g * GT
            fin = self.down_proj(self.ycT[:, :, t0:t0 + GT], FC, self.w_out[l], md(5), 1.0, self.xr[t0:t0 + GT, :], self.xr[t0:t0 + GT, :], GT, FC * GT // 2, [])
            self.phase_end(fin)

    def tm_proj(self, wsrc, col0, ncols, dst, odt, hT, NTg, o_w, o_q, pre_waits):
        c, P = self.c, self.P
        D = c["D"]
        KC = D // 128
        NTT = NTg // 128
        wv = wsrc.rearrange("(kc p) f -> p kc f", p=128)
        wbuf = self.b(o_w, KC * ncols).rearrange("p (a b) -> p a b", a=KC)
        of = [self.f(o_q + i * 512, ncols) for i in range(2)]
        ob = [self.b(o_q + i * 512, ncols) for i in range(2)]
        s_w, s_mm, s_a, s_ao = self.s_w, self.s_mm, self.s_a, self.s_ao
        for s, v in pre_waits:
            P.wait("pool", s, v)
            P.wait("act", s, v)
        KG = min(8, KC)
        for kg in range(KC // KG):
            P.op("pool", lambda e, kg=kg: e.dma_start(out=wbuf[:, kg * KG:(kg + 1) * KG, :], in_=wv[:, kg * KG:(kg + 1) * KG, col0:col0 + ncols]), s_w, dma=True)
        vw = s_w.n
        P.wait("pe", s_w, vw)
        a_vals, ao_vals = [], []
        for tt in range(NTT):
            bk = self.banks[tt % 2]
            if tt >= 2:
                P.wait("pe", s_a, a_vals[tt - 2])
            for kc in range(KC):
                fn = lambda e, bk=bk, kc=kc, tt=tt: e.matmul(bk[:, 0:ncols], hT[:, kc, tt * 128:(tt + 1) * 128], wbuf[:, kc, :], start=(kc == 0), stop=(kc == KC - 1))
                if kc == KC - 1:
                    vmm = P.op("pe", fn, s_mm)
                else:
                    P.op("pe", fn)
            P.wait("act", s_mm, vmm)
            if tt >= 2:
                P.wait("act", s_ao, ao_vals[tt - 2])
            dv = (of if odt == F32 else ob)[tt % 2]
            va = P.op("act", lambda e, dv=dv, bk=bk: e.copy(out=dv, in_=bk[:, 0:ncols]), s_a)
            a_vals.append(va)
            P.wait("sp", s_a, va)
            vao = P.op("sp", lambda e, dv=dv, tt=tt: e.dma_start(out=dst[tt * 128:(tt + 1) * 128, :], in_=dv), s_ao, dma=True)
            ao_vals.append(vao)
        return [(s_ao, ao_vals[-1]), (s_mm, vmm), (s_a, a_vals[-1])]

    def exchange(self, l):
        c, P = self.c, self.P
        T, CW, NCG, IDIM = c["T"], c["CW"], c["NCG"], c["IDIM"]
        KVW = self.KVW
        PAD = c["CK"] - 1
        NTL = T // 128
        s_l, s_d, s_st = self.s_m1, self.s_m2, self.s_m5
        srcb = self.b(0, 2 * T)
        srcf = self.f(T, NTL * PAD + 8)
        tmp = [self.f(2 * T + 64 + i * T, T) for i in range(NCG)]
        zt = self.f(2 * T + 64 + NCG * T, PAD + 2)

        def item(m, load_fn, src_view, tmp_view, store_fn):
            P.wait("sp", s_d, s_d.n)
            vl = P.op("sp", load_fn, s_l, dma=True)
            P.wait("dve", s_l, vl)
            P.wait("dve", s_st, s_st.n)
            for i in range(NCG):
                vd = P.op("dve", lambda e, i=i: e.tensor_scalar(out=tmp_view(i), in0=src_view, scalar1=self.onehot[0:m, i:i + 1], scalar2=None, op0=ALU.mult), s_d)
            P.wait("act", s_d, vd)
            for i in range(NCG):
                P.op("act", lambda e, i=i: store_fn(e, i), s_st, dma=True)

        v0 = P.op("dve", lambda e: e.memset(zt, 0.0), s_d)
        P.wait("act", s_d, v0)
        for cc in range(CW // 128):
            P.op("act", lambda e, cc=cc: e.dma_start(out=self.XH[cc * 128:(cc + 1) * 128, 0, :], in_=zt[:, 0:PAD]), s_st, dma=True)
        for cc in range(CW // 128):
            rows = slice(cc * 128, (cc + 1) * 128)
            hs = srcf[:, 0:NTL * PAD].rearrange("p (j t) -> p j t", t=PAD)
            item(128, lambda e, rows=rows, hs=hs: e.dma_start(out=hs, in_=self.uT[rows, :].rearrange("p (j t) -> p j t", t=128)[:, :, 128 - PAD:128]),
                 hs, lambda i: tmp[i][:, 0:NTL * PAD].rearrange("p (j t) -> p j t", t=PAD),
                 lambda e, i, rows=rows: e.dma_start(out=self.XH[rows, 1:, :].rearrange("p (j i) t -> p j i t", i=NCG)[:, :, i, :],
                                                     in_=tmp[i][:, 0:NTL * PAD].rearrange("p (j t) -> p j t", t=PAD)))
        self._halo_st = s_st.n
        for a in range(KVW // 128):
            rows = slice(a * 128, (a + 1) * 128)
            item(128, lambda e, rows=rows: e.dma_start(out=srcb[:, 0:T], in_=self.kT[rows, :]),
                 srcb[:, 0:T], lambda i: tmp[i],
                 lambda e, i, rows=rows: e.dma_start(out=self.XK[rows, :].rearrange("p (j i t) -> p j i t", i=NCG, t=128)[:, :, i, :],
                                                     in_=tmp[i].rearrange("p (j t) -> p j t", t=128)))
        item(IDIM, lambda e: e.dma_start(out=srcb[0:IDIM, 0:T], in_=self.kiT),
             srcb[0:IDIM, 0:T], lambda i: tmp[i][0:IDIM, :],
             lambda e, i: e.dma_start(out=self.XKI.rearrange("p (j i t) -> p j i t", i=NCG, t=128)[:, :, i, :],
                                      in_=tmp[i][0:IDIM, :].rearrange("p (j t) -> p j t", t=128)))
        for j in range(NTL):
            item(128, lambda e, j=j: e.dma_start(out=srcb[:, 0:KVW], in_=self.vS[j * 128:(j + 1) * 128, :]),
                 srcb[:, 0:KVW], lambda i: tmp[i][:, 0:KVW],
                 lambda e, i, j=j: e.dma_start(out=self.XV[(NCG * j + i) * 128:(NCG * j + i + 1) * 128, :], in_=tmp[i][:, 0:KVW]))
        vx_halo = None
        for src, dst in ((self.XH, self.RH), (self.XK, self.RK), (self.XV, self.RV), (self.XKI, self.RKI)):
            P.wait("pool", s_st, self._halo_st if src is self.XH else s_st.n)
            R0 = src.shape[0]
            total = 4
            for d in src.shape:
                total *= d
            pieces = max(1, -(-total // (3 << 20)))
            while R0 % pieces:
                pieces += 1
            rp = R0 // pieces
            for pi in range(pieces):
                vx = P.op("pool", lambda e, src=src, dst=dst, pi=pi, rp=rp: e.collective_compute(
                    "AllReduce", ALU.add, replica_groups=self.RG, ins=[src[pi * rp:(pi + 1) * rp].opt()], outs=[dst[pi * rp:(pi + 1) * rp].opt()]), self.s_x1)
                P.wait("pool", self.s_x1, vx)
            if vx_halo is None:
                vx_halo = vx
        self.phase_end([(self.s_x1, vx_halo), (s_st, s_st.n)])

    def conv(self, l):
        c, P = self.c, self.P
        T, CW, CK = c["T"], c["CW"], c["CK"]
        NCC = CW // 128
        PAD = CK - 1
        o = 0
        S1 = self.f(o, T); o += T
        S2 = self.f(o, T); o += T
        S3 = self.f(o, T); o += T
        NTL = T // 128
        NCG = c["NCG"]
        SEG = PAD + 128
        ubuf = self.f(o, NTL * SEG); o += NTL * SEG
        ub3 = ubuf.rearrange("p (j t) -> p j t", t=SEG)
        hc = self.f(o, NTL * NCG * PAD).rearrange("p (j i t) -> p j i t", i=NCG, t=PAD); o += NTL * NCG * PAD
        ybuf = self.f(o, T); o += T
        yb3 = ybuf.rearrange("p (j t) -> p j t", t=128)
        zbuf = self.f(o, T); o += T
        meanb = self.f(o, T); o += T
        rstdb = self.f(o, T); o += T
        zcb = self.b(o, T); o += T // 2
        cw = self.small[:, 64:64 + CK]
        cv = self.small[:, 100:104]
        s_l, s_d, s_a, s_p, s_st = self.s_m1, self.s_m2, self.s_m3, self.s_m4, self.s_m5
        NTB = (T + 511) // 512
        TBW = min(512, T)

        def chain(eng, fn):
            v = P.op(eng, fn, s_d if eng == "dve" else s_a)
            P.wait(eng, s_d if eng == "dve" else s_a, v)
            return v

        chain("dve", lambda e: e.memset(S1, 0.0))
        chain("dve", lambda e: e.memset(S2, 0.0))
        chain("dve", lambda e: e.memset(S3, 0.0))
        st_prev = 0
        for cc in range(NCC):
            rows = slice(cc * 128, (cc + 1) * 128)
            P.wait("sp", s_d, s_d.n)
            P.wait("sp", s_a, s_a.n)
            P.op("sp", lambda e, rows=rows: e.dma_start(out=ub3[:, :, PAD:SEG], in_=self.uT[rows, :].rearrange("p (j t) -> p j t", t=128)), s_l, dma=True)
            P.op("sp", lambda e, rows=rows: e.dma_start(out=hc, in_=self.RH[rows, 0:NTL * NCG, :].rearrange("p (j i) t -> p j i t", i=NCG)), s_l, dma=True)
            P.op("sp", lambda e, rows=rows: e.dma_start(out=cw, in_=self.conv_w[l][:, rows].rearrange("k c -> c k"), allow_slow_non_contiguous=True), s_l, dma=True)
            vl = P.op("sp", lambda e, rows=rows: e.dma_start(out=cv, in_=self.cvec[l][:, rows].rearrange("k c -> c k"), allow_slow_non_contiguous=True), s_l, dma=True)
            P.wait("dve", s_l, vl)
            P.wait("dve", s_st, st_prev)
            chain("dve", lambda e: e.tensor_scalar(out=ub3[:, :, 0:PAD], in0=hc[:, :, 0, :], scalar1=self.onehot[:, 0:1], scalar2=None, op0=ALU.mult))
            for i in range(1, NCG):
                chain("dve", lambda e, i=i: e.scalar_tensor_tensor(out=ub3[:, :, 0:PAD], in0=hc[:, :, i, :], scalar=self.onehot[:, i:i + 1], in1=ub3[:, :, 0:PAD], op0=ALU.mult, op1=ALU.add))
            chain("dve", lambda e: e.tensor_scalar(out=yb3, in0=ub3[:, :, 0:128], scalar1=cw[:, 0:1], scalar2=cv[:, 0:1], op0=ALU.mult, op1=ALU.add))
            for j in range(1, CK):
                chain("dve", lambda e, j=j: e.scalar_tensor_tensor(out=yb3, in0=ub3[:, :, j:j + 128], scalar=cw[:, j:j + 1], in1=yb3, op0=ALU.mult, op1=ALU.add))
            vy = s_d.n
            P.wait("sp", s_d, vy)
            st_prev = P.op("sp", lambda e, rows=rows: e.dma_start(out=self.yT[rows, :], in_=ybuf), s_st, dma=True)
            P.wait("act", s_d, vy)
            chain("act", lambda e: e.activation(out=zbuf, in_=ybuf, func=AF.Square))
            chain("dve", lambda e: e.tensor_tensor(out=S1, in0=S1, in1=ybuf, op=ALU.add))
            P.wait("dve", s_a, s_a.n)
            chain("dve", lambda e: e.tensor_tensor(out=S2, in0=S2, in1=zbuf, op=ALU.add))
        P.wait("pe", s_d, s_d.n)
        for tb in range(NTB):
            cs = slice(tb * TBW, (tb + 1) * TBW)
            P.wait("pe", s_a, s_a.n)
            v1 = P.op("pe", lambda e, cs=cs: e.matmul(self.banks[0][:, 0:TBW], self.ones[:], S1[:, cs], start=True, stop=True), s_p)
            v2 = P.op("pe", lambda e, cs=cs: e.matmul(self.banks[1][:, 0:TBW], self.ones[:], S2[:, cs], start=True, stop=True), s_p)
            P.wait("act", s_p, v2)
            chain("act", lambda e, cs=cs: e.mul(out=meanb[:, cs], in_=self.banks[0][:, 0:TBW], mul=1.0 / CW))
            chain("act", lambda e, cs=cs: e.mul(out=rstdb[:, cs], in_=self.banks[1][:, 0:TBW], mul=1.0 / CW))
        P.wait("dve", s_a, s_a.n)
        chain("dve", lambda e: e.tensor_tensor(out=zbuf, in0=meanb, in1=meanb, op=ALU.mult))
        chain("dve", lambda e: e.tensor_tensor(out=rstdb, in0=rstdb, in1=zbuf, op=ALU.subtract))
        P.wait("act", s_d, s_d.n)
        chain("act", lambda e: e.activation(out=rstdb, in_=rstdb, func=AF.Sqrt, bias=self.epsb))
        P.wait("dve", s_a, s_a.n)
        chain("dve", lambda e: e.reciprocal(out=rstdb, in_=rstdb))
        for cc in range(NCC):
            rows = slice(cc * 128, (cc + 1) * 128)
            P.wait("sp", s_d, s_d.n)
            P.wait("sp", s_a, s_a.n)
            P.wait("sp", s_st, st_prev)
            P.op("sp", lambda e, rows=rows: e.dma_start(out=ybuf, in_=self.yT[rows, :]), s_l, dma=True)
            vl = P.op("sp", lambda e, rows=rows: e.dma_start(out=cv, in_=self.cvec[l][:, rows].rearrange("k c -> c k"), allow_slow_non_contiguous=True), s_l, dma=True)
            P.wait("dve", s_l, vl)
            chain("dve", lambda e: e.tensor_tensor(out=ybuf, in0=ybuf, in1=meanb, op=ALU.subtract))
            chain("dve", lambda e: e.tensor_tensor(out=ybuf, in0=ybuf, in1=rstdb, op=ALU.mult))
            P.wait("act", s_d, s_d.n)
            chain("act", lambda e: e.activation(out=zbuf, in_=ybuf, func=AF.Silu, scale=cv[:, 1:2], bias=cv[:, 2:3]))
            chain("act", lambda e: e.activation(out=ybuf, in_=zbuf, func=AF.Square))
            P.wait("dve", s_a, s_a.n)
            chain("dve", lambda e: e.tensor_tensor(out=S3, in0=S3, in1=ybuf, op=ALU.add))
            chain("dve", lambda e: e.tensor_scalar(out=zcb, in0=zbuf, scalar1=cv[:, 3:4], scalar2=None, op0=ALU.mult))
            P.wait("sp", s_d, s_d.n)
            st_prev = P.op("sp", lambda e, cc=cc: e.dma_start(out=self.ycT[cc], in_=zcb), s_st, dma=True)
        P.wait("pe", s_d, s_d.n)
        for tb in range(NTB):
            cs = slice(tb * TBW, (tb + 1) * TBW)
            P.wait("pe", s_a, s_a.n)
            v1 = P.op("pe", lambda e, cs=cs: e.matmul(self.banks[0][:, 0:TBW], self.ones[:], S3[:, cs], start=True, stop=True), s_p)
            P.wait("act", s_p, v1)
            chain("act", lambda e, cs=cs: e.activation(out=rstdb[:, cs], in_=self.banks[0][:, 0:TBW], func=AF.Sqrt, scale=1.0 / CW, bias=self.epsb))
        P.wait("dve", s_a, s_a.n)
        chain("dve", lambda e: e.reciprocal(out=rstdb, in_=rstdb))
        for cc in range(NCC):
            P.wait("sp", s_d, s_d.n)
            P.wait("sp", s_st, st_prev)
            vl = P.op("sp", lambda e, cc=cc: e.dma_start(out=zcb, in_=self.ycT[cc]), s_l, dma=True)
            P.wait("dve", s_l, vl)
            chain("dve", lambda e: e.tensor_tensor(out=zcb, in0=zcb, in1=rstdb, op=ALU.mult))
            P.wait("sp", s_d, s_d.n)
            st_prev = P.op("sp", lambda e, cc=cc: e.dma_start(out=self.ycT[cc], in_=zcb), s_st, dma=True)
        self.phase_end([(s_st, st_prev)])

    def attn(self, l):
        c, P = self.c, self.P
        T, CW, AW, NH, NKV, IH, IDIM, TOPK = c["T"], c["CW"], c["AW"], c["NH"], c["NKV"], c["IH"], c["IDIM"], c["TOPK"]
        GRP = NH // NKV
        NCG = c["NCG"]
        TL = T
        T = self.TK
        NT = T // 128
        NTL = TL // 128
        scale = 128 ** -0.5
        o = 0
        kT_sb = self.b(o, NKV * T).rearrange("p (a b) -> p a b", a=NKV); o += NKV * T // 2
        VW = NKV * 130
        v_sb = self.b(o, NT * VW).rearrange("p (a b d) -> p a b d", a=NT, b=NKV); o += NT * VW // 2
        kiT_sb = self.b(o, T); o += T // 2
        an_b = self.f(o, AW); o += AW
        scores = self.f(o, T); o += T
        o_wk = o
        wk = self.f(o, T); o += T
        sel = self.b(o, T); o += T // 2
        pbuf = self.b(o, T); o += T // 2
        pmbuf = self.b(o, T); o += T // 2
        pT = self.b(o, NT * 128).rearrange("p (a b) -> p a b", a=NT); o += NT * 64
        yat = self.f(o, AW); o += AW
        ynb = pbuf[:, 0:AW]
        yaT = self.b(o, AW).rearrange("p (a b) -> p a b", a=AW // 128); o += AW // 2
        qT_t = self.b(o, NH * 128).rearrange("p (a b) -> p a b", a=NH); o += NH * 64
        qiT_t = self.b(o, IH * 128).rearrange("p (a b) -> p a b", a=IH); o += IH * 64
        o_dall = o
        Dall = self.b(o, IH * 128).rearrange("p (a b) -> p a b", a=IH); o += IH * 64
        abuf = [self.b(o + i * 256, 512) for i in range(3)]; o += 768
        if IH >= NT:
            pT2 = self.b(o_dall, NT * 128).rearrange("p (a b) -> p a b", a=NT)
        else:
            pT2 = self.b(o, NT * 128).rearrange("p (a b) -> p a b", a=NT); o += NT * 64
        pbs = [pbuf, self.b(o_wk, T)]
        pms = [pmbuf, self.b(o_wk + T // 2, T)]
        pTs = [pT, pT2]
        rinv2 = [self.small[:, 211:212], self.small[:, 212:213]]
        s_L, s_E, s_M, s_T, s_V, s_PV, s_Y = self.s_w, self.s_mm, self.s_sg, self.s_a, self.s_ao, self.s_al, self.s_wd
        lgb = self.banks[0:4]
        tpbs = [self.banks[4][:].bitcast(BF16), self.banks[5][:].bitcast(BF16)]
        pob = [self.banks[6], self.banks[7]]
        wi_t = self.small[:, 128:128 + IH]
        wib = self.small[:, 160:160 + IH].bitcast(BF16)
        m8 = self.small[:, 200:208]
        thr = self.small[:, 208:209]
        ssa = self.small[:, 209:210]
        ra = self.small[:, 210:211]
        rinv = self.small[:, 211:212]
        s_l, s_d, s_a, s_p, s_st, s_q = self.s_m1, self.s_m2, self.s_m3, self.s_m4, self.s_m5, self.s_m6
        banks = self.banks
        tpb = banks[6][:].bitcast(BF16)

        def chain(eng, fn):
            sm = {"dve": s_d, "act": s_a, "pe": s_p}[eng]
            v = P.op(eng, fn, sm)
            P.wait(eng, sm, v)
            return v

        for a in range(NKV):
            P.op("pool", lambda e, a=a: e.dma_start(out=kT_sb[:, a, :], in_=self.RK[a * 128:(a + 1) * 128, :]), s_l, dma=True)
        for kt0 in range(0, NT, 8):
            n = min(8, NT - kt0)
            for kv in range(NKV):
                P.op("pool", lambda e, kt0=kt0, n=n, kv=kv: e.dma_start(
                    out=v_sb[:, kt0:kt0 + n, kv, 0:128],
                    in_=self.RV[kt0 * 128:(kt0 + n) * 128, kv * 128:(kv + 1) * 128].rearrange("(a p) d -> p a d", p=128)), s_l, dma=True)
        P.op("pool", lambda e: e.dma_start(out=kiT_sb[0:IDIM, :], in_=self.RKI), s_l, dma=True)
        vl = P.op("pool", lambda e: e.dma_start(out=an_b, in_=self.anorm[l:l + 1, :].partition_broadcast(128)), s_l, dma=True)
        P.wait("dve", s_l, vl)
        chain("dve", lambda e: e.memset(v_sb[:, :, :, 128:130], 1.0))
        chain("dve", lambda e: e.memset(ssa, 0.0))
        P.wait("pe", s_l, vl)
        P.wait("pe", s_d, s_d.n)
        st_prev = 0
        for g in range(NTL):
            S = (NCG * g + NCG) * 128
            nkt = S // 128
            nkb = (S + 511) // 512
            ts = slice(g * 128, (g + 1) * 128)
            dsl = slice(NCG * g * 128, S)
            P.wait("sp", s_p, s_p.n)
            P.wait("sp", s_d, s_d.n)
            P.op("sp", lambda e, ts=ts: e.dma_start(out=qT_t, in_=self.qT[:, ts].rearrange("(h p) t -> p h t", p=128)), s_q, dma=True)
            P.op("sp", lambda e, ts=ts: e.dma_start(out=qiT_t[0:IDIM, :, :], in_=self.qiT[:, ts].rearrange("(h p) t -> p h t", p=IDIM)), s_q, dma=True)
            vq = P.op("sp", lambda e, ts=ts: e.dma_start(out=wi_t, in_=self.wiS[ts, :]), s_q, dma=True)
            P.wait("dve", s_q, vq)
            for h in range(IH):
                P.op("dve", lambda e, h=h: e.tensor_scalar(out=Dall[:, h, :], in0=self.identb[:], scalar1=wi_t[:, h:h + 1], scalar2=None, op0=ALU.mult))
            chain("dve", lambda e: e.memset(thr, 0.5 * NEG))
            P.wait("pe", s_q, vq)
            P.wait("pe", s_d, s_d.n)
            units = [(kb, h) for kb in range(nkb) for h in range(IH)]
            NU = len(units)
            a_hist = []
            sc_ev = {}
            for u in range(NU + 2):
                if u < NU:
                    kb, h = units[u]
                    w = min(512, S - kb * 512)
                    ks = slice(kb * 512, kb * 512 + w)
                    r = u % 3
                    if u >= 3:
                        P.wait("pe", s_a, a_hist[u - 3])
                    v1 = P.op("pe", lambda e, r=r, h=h, ks=ks, w=w: e.matmul(banks[r][:, 0:w], qiT_t[0:IDIM, h, :], kiT_sb[0:IDIM, ks], start=True, stop=True), s_p)
                    P.wait("act", s_p, v1)
                    va = P.op("act", lambda e, r=r, w=w: e.activation(out=abuf[r][:, 0:w], in_=banks[r][:, 0:w], func=AF.Relu), s_a)
                    a_hist.append(va)
                if u >= 2:
                    u2 = u - 2
                    kb, h = units[u2]
                    w = min(512, S - kb * 512)
                    ks = slice(kb * 512, kb * 512 + w)
                    r = u2 % 3
                    scb = banks[3 + kb % 2]
                    P.wait("pe", s_a, a_hist[u2])
                    if h == 0 and kb >= 2:
                        P.wait("pe", s_a, sc_ev[kb - 2])
                    vm2 = P.op("pe", lambda e, r=r, h=h, w=w, scb=scb: e.matmul(scb[:, 0:w], Dall[:, h, :], abuf[r][:, 0:w], start=(h == 0), stop=(h == IH - 1)), s_p)
                    if h == IH - 1:
                        P.wait("act", s_p, vm2)
                        sc_ev[kb] = P.op("act", lambda e, ks=ks, w=w, scb=scb: e.copy(out=scores[:, ks], in_=scb[:, 0:w]), s_a)
            P.wait("dve", s_a, s_a.n)
            chain("dve", lambda e, dsl=dsl: e.tensor_tensor(out=scores[:, dsl], in0=scores[:, dsl], in1=self.cmask[:], op=ALU.add))
            if S > TOPK:
                chain("dve", lambda e, S=S: e.tensor_copy(out=wk[:, 0:S], in_=scores[:, 0:S]))
                for r in range(TOPK // 8):
                    chain("dve", lambda e, S=S: e.max(out=m8, in_=wk[:, 0:S]))
                    if r < TOPK // 8 - 1:
                        chain("dve", lambda e, S=S: e.match_replace(out=wk[:, 0:S], in_to_replace=m8, in_values=wk[:, 0:S], imm_value=NEG))
                chain("dve", lambda e: e.tensor_reduce(out=thr, in_=m8, axis=mybir.AxisListType.X, op=ALU.min))
                chain("dve", lambda e: e.tensor_scalar(out=thr, in0=thr, scalar1=0.5 * NEG, scalar2=None, op0=ALU.max))
            chain("dve", lambda e, S=S: e.tensor_scalar(out=sel[:, 0:S], in0=scores[:, 0:S], scalar1=thr, scalar2=None, op0=ALU.is_ge))
            if g == 0:
                Lc, Tc = [0], [0]
                E_hist, V_hist = [], []
                M_val, T_last, PV_val, Y_val = {}, {}, {}, {}
                hbase = 0
            for step in range(NH + 1):
                if step < NH:
                    h = step
                    H = hbase + h
                    bsel = H % 2
                    kv = h // GRP
                    for kb in range(nkb):
                        w = min(512, S - kb * 512)
                        ks = slice(kb * 512, kb * 512 + w)
                        slot = Lc[0] % 4
                        if Lc[0] >= 4:
                            P.wait("pe", s_E, E_hist[Lc[0] - 4])
                        if step == 0 and kb == 0:
                            P.wait("pe", s_d, s_d.n)
                        vL = P.op("pe", lambda e, slot=slot, h=h, kv=kv, ks=ks, w=w: e.matmul(lgb[slot][:, 0:w], qT_t[:, h, :], kT_sb[:, kv, ks], start=True, stop=True), s_L)
                        P.wait("act", s_L, vL)
                        if kb == 0 and H >= 2:
                            P.wait("act", s_M, M_val[H - 2])
                        vE = P.op("act", lambda e, slot=slot, bsel=bsel, ks=ks, w=w: e.activation(out=pbs[bsel][:, ks], in_=lgb[slot][:, 0:w], func=AF.Exp, scale=scale), s_E)
                        E_hist.append(vE)
                        Lc[0] += 1
                    P.wait("dve", s_E, vE)
                    if H >= 2:
                        P.wait("dve", s_T, T_last[H - 2])
                    M_val[H] = P.op("dve", lambda e, bsel=bsel, S=S: e.tensor_tensor(out=pms[bsel][:, 0:S], in0=pbs[bsel][:, 0:S], in1=sel[:, 0:S], op=ALU.mult), s_M)
                if step >= 1:
                    h = step - 1
                    H = hbase + h
                    bsel = H % 2
                    kv = h // GRP
                    first = True
                    for k0 in range(0, nkt, 8):
                        n = min(8, nkt - k0)
                        tsl = Tc[0] % 2
                        P.wait("pe", s_M, M_val[H])
                        if Tc[0] >= 2:
                            P.wait("pe", s_V, V_hist[Tc[0] - 2])
                        for i in range(n):
                            fn = lambda e, i=i, k0=k0, tsl=tsl, bsel=bsel: e.transpose(out=tpbs[tsl][:, i * 128:(i + 1) * 128], in_=pms[bsel][:, (k0 + i) * 128:(k0 + i + 1) * 128], identity=self.identb[:])
                            if i == n - 1:
                                vT = P.op("pe", fn, s_T)
                            else:
                                P.op("pe", fn)
                        P.wait("act", s_T, vT)
                        if first and H >= 2:
                            P.wait("act", s_PV, PV_val[H - 2])
                        first = False
                        vV = P.op("act", lambda e, k0=k0, n=n, tsl=tsl, bsel=bsel: e.copy(out=pTs[bsel][:, k0:k0 + n, :], in_=tpbs[tsl][:, 0:n * 128].rearrange("p (a b) -> p a b", a=n)), s_V)
                        V_hist.append(vV)
                        Tc[0] += 1
                    T_last[H] = vT
                    P.wait("pe", s_V, vV)
                    if H >= 2:
                        P.wait("pe", s_Y, Y_val[H - 2])
                    for kt in range(nkt):
                        fn = lambda e, kt=kt, kv=kv, nkt=nkt, bsel=bsel: e.matmul(pob[bsel][:, 0:129], pTs[bsel][:, kt, :], v_sb[:, kt, kv, 0:129], start=(kt == 0), stop=(kt == nkt - 1))
                        if kt == nkt - 1:
                            vPV = P.op("pe", fn, s_PV)
                        else:
                            P.op("pe", fn)
                    PV_val[H] = vPV
                    P.wait("dve", s_PV, vPV)
                    vr = P.op("dve", lambda e, bsel=bsel: e.reciprocal(out=rinv2[bsel], in_=pob[bsel][:, 128:129]), s_Y)
                    P.wait("dve", s_Y, vr)
                    Y_val[H] = P.op("dve", lambda e, h=h, bsel=bsel: e.tensor_scalar(out=yat[:, h * 128:(h + 1) * 128], in0=pob[bsel][:, 0:128], scalar1=rinv2[bsel], scalar2=None, op0=ALU.mult), s_Y)
            hbase += NH
            P.wait("act", s_Y, Y_val[hbase - 1])
            P.wait("dve", s_Y, Y_val[hbase - 1])
            vfl = P.op("dve", lambda e: e.memset(thr, 0.5 * NEG), s_d)
            P.wait("dve", s_d, vfl)
            P.wait("pe", s_d, vfl)
            P.wait("sp", s_d, vfl)
            P.wait("act", s_d, s_d.n)
            P.wait("act", s_st, st_prev)
            chain("act", lambda e: e.activation(out=ynb, in_=yat, func=AF.Square, accum_out=ssa))
            chain("act", lambda e: e.activation(out=ra, in_=ssa, func=AF.Sqrt, scale=1.0 / AW, bias=self.epsb))
            P.wait("dve", s_a, s_a.n)
            chain("dve", lambda e: e.reciprocal(out=ra, in_=ra))
            chain("dve", lambda e: e.scalar_tensor_tensor(out=ynb, in0=yat, scalar=ra, in1=an_b, op0=ALU.mult, op1=ALU.mult))
            chain("dve", lambda e: e.memset(ssa, 0.0))
            P.wait("pe", s_d, s_d.n)
            for k0 in range(0, AW // 128, 8):
                n = min(8, AW // 128 - k0)
                P.wait("pe", s_a, s_a.n)
                for i in range(n):
                    v1 = P.op("pe", lambda e, i=i, k0=k0: e.transpose(out=tpb[:, i * 128:(i + 1) * 128], in_=ynb[:, (k0 + i) * 128:(k0 + i + 1) * 128], identity=self.identb[:]), s_p)
                P.wait("act", s_p, v1)
                chain("act", lambda e, k0=k0, n=n: e.copy(out=yaT[:, k0:k0 + n, :], in_=tpb[:, 0:n * 128].rearrange("p (a b) -> p a b", a=n)))
            P.wait("sp", s_a, s_a.n)
            st_prev = P.op("sp", lambda e, ts=ts: e.dma_start(out=self.ycT[CW // 128:, :, ts].rearrange("a p t -> p a t"), in_=yaT), s_st, dma=True)
        self.phase_end([(s_st, st_prev)])


_CACHE = {}


def _run(cfg, inputs, ncores=None):
    key = tuple(sorted(cfg.items()))
    if key not in _CACHE:
        _CACHE[key] = MK(cfg).build()
    nc = _CACHE[key]
    NCG, NB, TL, D = cfg["NCG"], cfg["NB"], cfg["T"], cfg["D"]
    NTL = TL // 128
    W = 9 * D // NCG
    ident = np.eye(128, dtype=np.float32)
    tri = np.where(np.arange(128)[None, :] <= np.arange(128)[:, None], 0.0, NEG).astype(np.float32)
    norms = np.ascontiguousarray(np.stack([inputs["ffn1_norm"], inputs["mix_norm"], inputs["ffn2_norm"]], axis=1))
    cvec = np.ascontiguousarray(np.stack([inputs["conv_b"], inputs["conv_ln_g"], inputs["conv_ln_b"], inputs["conv_out_norm"]], axis=1))
    shared = dict(norms=norms, ffn1_wgu=inputs["ffn1_wgu"], ffn2_wgu=inputs["ffn2_wgu"],
                  ffn1_wd=inputs["ffn1_wd"], ffn2_wd=inputs["ffn2_wd"], w_in=inputs["w_in"], conv_w=inputs["conv_w"], cvec=cvec,
                  attn_out_norm=inputs["attn_out_norm"], w_out=inputs["w_out"], final_norm=inputs["final_norm"].reshape(1, -1),
                  ident=ident)
    in_maps = []
    for b in range(NB):
        xb = inputs["x"][b].reshape(NTL, NCG, 128, D)
        for q in range(NCG):
            m = dict(shared)
            m["x"] = np.ascontiguousarray(xb[:, q]).reshape(TL, D)
            m["c"] = np.ascontiguousarray(inputs["c"][b:b + 1])
            m["ada_w"] = np.ascontiguousarray(inputs["ada_w"][:, :, q * W:(q + 1) * W])
            m["ada_b"] = np.ascontiguousarray(inputs["ada_b"][:, q * W:(q + 1) * W])
            oh = np.zeros((128, NCG), np.float32)
            oh[:, q] = 1.0
            m["onehot"] = oh
            cm = np.full((128, NCG, 128), NEG, np.float32)
            cm[:, :q, :] = 0.0
            cm[:, q, :] = tri
            m["cmask"] = cm.reshape(128, NCG * 128)
            in_maps.append(m)
    res = run_bass_kernel_spmd(nc, in_maps, core_ids=list(range(NB * NCG)))
    out = np.empty((NB, NTL, NCG, 128, D), np.float32)
    for b in range(NB):
        for q in range(NCG):
            out[b, :, q] = res.results[b * NCG + q]["out"].reshape(NTL, 128, D)
    return out.reshape(NB, NTL * NCG * 128, D)


def kernel(**inputs):
    inputs = {k: np.asarray(v) for k, v in inputs.items()}
    return _run(FULL, inputs)
```
